# Optimizing a Trainium2 kernel written in Bass

```python
import jax, jax.numpy as jnp
from jax import lax
import numpy as np

D_MODEL = 1024
BATCH = 4
SEQ = 4096
DEPTH = 1
DEC_BATCH = 32
DEC_SEQ = 4
PAST_LEN = 8192
PAGE_SIZE = 128

LRU_WIDTH = D_MODEL // 2
LRU_BLOCKS = 8
LRU_BLOCK_DIM = LRU_WIDTH // LRU_BLOCKS
CONV_W = 4
LRU_C = 8.0
HEAD_DIM = 64
SB_HEADS = (D_MODEL // 2) // HEAD_DIM
SB_WIDTH = SB_HEADS * HEAD_DIM
MIX_WIDTH = LRU_WIDTH + SB_WIDTH
IN_COLS = 2 * LRU_WIDTH + 3 * SB_WIDTH
Q_BLOCK = 128
SB_BIAS_INIT = -8.0
N_GROUPS = 4
EXPERTS_PER_GROUP = 8
N_EXPERTS = N_GROUPS * EXPERTS_PER_GROUP
TOP_K_IN_GROUP = 2
D_EXPERT = D_MODEL // 2
EPS = 1e-6

kernel_name = "hymba_rglru_stickbreaking_hmoe_step"


def rmsnorm(x, g):
    xf = x.astype(jnp.float32)
    y = xf * lax.rsqrt(jnp.mean(xf * xf, axis=-1, keepdims=True) + EPS)
    return (y * g.astype(jnp.float32)).astype(x.dtype)


def causal_conv(u, buf, w, b):
    T = u.shape[1]
    up = jnp.concatenate([buf.astype(u.dtype), u], axis=1)
    out = b[None, None, :] + sum(w[k][None, None, :] * up[:, k:k + T] for k in range(CONV_W))
    return out, up[:, -(CONV_W - 1):]


def rg_lru(u, h0, w_a, b_a, w_i, b_i, lam):
    B, T, C = u.shape
    uf = u.astype(jnp.float32)
    ub = uf.reshape(B, T, LRU_BLOCKS, LRU_BLOCK_DIM)
    r = jax.nn.sigmoid(jnp.einsum('btnd,nde->btne', ub, w_a.astype(jnp.float32)).reshape(B, T, C) + b_a.astype(jnp.float32))
    i = jax.nn.sigmoid(jnp.einsum('btnd,nde->btne', ub, w_i.astype(jnp.float32)).reshape(B, T, C) + b_i.astype(jnp.float32))
    log_a = -LRU_C * r * jax.nn.softplus(-lam.astype(jnp.float32))
    a = jnp.exp(log_a)
    xin = jnp.sqrt(-jnp.expm1(2.0 * log_a)) * (i * uf)

    def step(h, inp):
        a_t, x_t = inp
        h = a_t * h + x_t
        return h, h

    h_T, hs = lax.scan(step, h0.astype(jnp.float32), (a.transpose(1, 0, 2), xin.transpose(1, 0, 2)))
    return hs.transpose(1, 0, 2), h_T


def sb_block(q, k, v, q_pos, k_pos, b_sb):
    z = jnp.einsum('bqhd,bshd->bhqs', q.astype(jnp.float32), k.astype(jnp.float32)) * (HEAD_DIM ** -0.5)
    z = z + b_sb.astype(jnp.float32)[None, :, None, None]
    mask = (k_pos[None, :] < q_pos[:, None])[None, None]
    log_1mb = jnp.where(mask, jax.nn.log_sigmoid(-z), 0.0)
    later = lax.cumsum(log_1mb, axis=3, reverse=True) - log_1mb
    wts = jnp.where(mask, jnp.exp(jax.nn.log_sigmoid(z) + later), 0.0)
    return jnp.einsum('bhqs,bshd->bqhd', wts, v.astype(jnp.float32))


def sb_attend(q, k, v, q_pos, k_pos, b_sb):
    B, T, H, Dh = q.shape
    qb = min(Q_BLOCK, T)
    nb = -(-T // qb)
    pad = nb * qb - T
    q = jnp.pad(q, ((0, 0), (0, pad), (0, 0), (0, 0)))
    q_pos = jnp.pad(q_pos, (0, pad))
    q_blocks = q.reshape(B, nb, qb, H, Dh).transpose(1, 0, 2, 3, 4)
    pos_blocks = q_pos.reshape(nb, qb)
    out = lax.map(lambda a: sb_block(a[0], k, v, a[1], k_pos, b_sb), (q_blocks, pos_blocks))
    return out.transpose(1, 0, 2, 3, 4).reshape(B, nb * qb, H, Dh)[:, :T]


def hier_moe(h, w_rg, b_rg, w_re, b_re, w_eg, w_eu, w_ed):
    B, T, D = h.shape
    t = h.reshape(B * T, D)
    g_logits = (t @ w_rg + b_rg).astype(jnp.float32)
    g_idx = jnp.argmax(g_logits, axis=-1)
    p_g = jnp.max(jax.nn.softmax(g_logits, axis=-1), axis=-1, keepdims=True)
    e_logits = (t @ w_re + b_re).astype(jnp.float32).reshape(-1, N_GROUPS, EXPERTS_PER_GROUP)
    in_group = jnp.einsum('ng,nge->ne', jax.nn.one_hot(g_idx, N_GROUPS, dtype=jnp.float32), e_logits)
    top_v, top_i = lax.top_k(in_group, TOP_K_IN_GROUP)
    w_sel = jax.nn.softmax(top_v, axis=-1) * p_g
    e_sel = g_idx[:, None] * EXPERTS_PER_GROUP + top_i
    combine = jnp.einsum('nk,nke->ne', w_sel, jax.nn.one_hot(e_sel, N_EXPERTS, dtype=jnp.float32))
    y = jnp.zeros((B * T, D), jnp.float32)
    for e in range(N_EXPERTS):
        a = jax.nn.silu(t @ w_eg[e]) * (t @ w_eu[e])
        y = y + combine[:, e:e + 1] * (a @ w_ed[e]).astype(jnp.float32)
    return y.reshape(B, T, D).astype(h.dtype)


def layer(x, lru_h0, conv_buf, k_past, v_past, pos0, p):
    B, T, _ = x.shape
    hn = rmsnorm(x, p['g_mix'])
    proj = jnp.einsum('btd,dc->btc', hn, p['w_in'])
    xl = proj[..., :LRU_WIDTH]
    gl = proj[..., LRU_WIDTH:2 * LRU_WIDTH]
    o = 2 * LRU_WIDTH
    q = proj[..., o:o + SB_WIDTH].reshape(B, T, SB_HEADS, HEAD_DIM)
    k = proj[..., o + SB_WIDTH:o + 2 * SB_WIDTH].reshape(B, T, SB_HEADS, HEAD_DIM)
    v = proj[..., o + 2 * SB_WIDTH:o + 3 * SB_WIDTH].reshape(B, T, SB_HEADS, HEAD_DIM)
    u, new_buf = causal_conv(xl, conv_buf, p['conv_w'], p['conv_b'])
    hs, h_T = rg_lru(u, lru_h0, p['w_a'], p['b_a'], p['w_i'], p['b_i'], p['lam'])
    y_lru = hs * jax.nn.gelu(gl.astype(jnp.float32))
    k_all = jnp.concatenate([k_past.astype(k.dtype), k], axis=1)
    v_all = jnp.concatenate([v_past.astype(v.dtype), v], axis=1)
    k_pos = jnp.arange(pos0 + T, dtype=jnp.int32)
    q_pos = pos0 + jnp.arange(T, dtype=jnp.int32)
    y_sb = sb_attend(q, k_all, v_all, q_pos, k_pos, p['b_sb']).reshape(B, T, SB_WIDTH)
    mix = jnp.concatenate([rmsnorm(y_lru, p['g_out_lru']), rmsnorm(y_sb, p['g_out_sb'])], axis=-1).astype(x.dtype)
    x = x + jnp.einsum('btc,cd->btd', mix, p['w_out'])
    x = x + hier_moe(rmsnorm(x, p['g_ffn']), p['w_rg'], p['b_rg'], p['w_re'], p['b_re'], p['w_eg'], p['w_eu'], p['w_ed'])
    return x, k, v, h_T, new_buf


def setup_inputs(seed: int = 0) -> dict:
    key = jax.random.key(seed)
    ks = jax.random.split(key, 32)
    f32 = jnp.float32
    n_pages = PAST_LEN // PAGE_SIZE
    n_pool = (DEC_BATCH * n_pages * 5) // 4
    nrm = lambda k, s, sc: jax.random.normal(k, s, f32) * sc
    perm = jax.random.permutation(ks[0], n_pool)[:DEC_BATCH * n_pages]
    u = jax.random.uniform(ks[1], (DEPTH, LRU_WIDTH), f32, 0.9, 0.999)
    a0 = u ** (1.0 / LRU_C)
    lam = jnp.log(a0) - jnp.log1p(-a0)
    return {
        "x_prompt": nrm(ks[2], (BATCH, SEQ, D_MODEL), 1.0),
        "x_sample": nrm(ks[3], (DEC_BATCH, DEC_SEQ, D_MODEL), 1.0),
        "cache_k": nrm(ks[4], (DEPTH, n_pool, PAGE_SIZE, SB_HEADS, HEAD_DIM), 1.0),
        "cache_v": nrm(ks[5], (DEPTH, n_pool, PAGE_SIZE, SB_HEADS, HEAD_DIM), 1.0),
        "state_lru_h": nrm(ks[6], (DEPTH, DEC_BATCH, LRU_WIDTH), 0.5),
        "state_conv": nrm(ks[7], (DEPTH, DEC_BATCH, CONV_W - 1, LRU_WIDTH), 1.0),
        "page_table": perm.reshape(DEC_BATCH, n_pages).astype(jnp.int32),
        "g_mix": 1.0 + nrm(ks[8], (DEPTH, D_MODEL), 0.02),
        "w_in": nrm(ks[9], (DEPTH, D_MODEL, IN_COLS), D_MODEL ** -0.5),
        "conv_w": nrm(ks[10], (DEPTH, CONV_W, LRU_WIDTH), CONV_W ** -0.5),
        "conv_b": nrm(ks[11], (DEPTH, LRU_WIDTH), 0.01),
        "w_a": nrm(ks[12], (DEPTH, LRU_BLOCKS, LRU_BLOCK_DIM, LRU_BLOCK_DIM), LRU_BLOCK_DIM ** -0.5),
        "b_a": nrm(ks[13], (DEPTH, LRU_WIDTH), 0.01),
        "w_i": nrm(ks[14], (DEPTH, LRU_BLOCKS, LRU_BLOCK_DIM, LRU_BLOCK_DIM), LRU_BLOCK_DIM ** -0.5),
        "b_i": nrm(ks[15], (DEPTH, LRU_WIDTH), 0.01),
        "lam": lam,
        "b_sb": SB_BIAS_INIT + nrm(ks[28], (DEPTH, SB_HEADS), 0.5),
        "g_out_lru": 1.0 + nrm(ks[16], (DEPTH, LRU_WIDTH), 0.02),
        "g_out_sb": 1.0 + nrm(ks[17], (DEPTH, SB_WIDTH), 0.02),
        "w_out": nrm(ks[18], (DEPTH, MIX_WIDTH, D_MODEL), MIX_WIDTH ** -0.5),
        "g_ffn": 1.0 + nrm(ks[19], (DEPTH, D_MODEL), 0.02),
        "w_rg": nrm(ks[20], (DEPTH, D_MODEL, N_GROUPS), D_MODEL ** -0.5),
        "b_rg": nrm(ks[21], (DEPTH, N_GROUPS), 0.01),
        "w_re": nrm(ks[22], (DEPTH, D_MODEL, N_EXPERTS), D_MODEL ** -0.5),
        "b_re": nrm(ks[23], (DEPTH, N_EXPERTS), 0.01),
        "w_eg": nrm(ks[24], (DEPTH, N_EXPERTS, D_MODEL, D_EXPERT), D_MODEL ** -0.5),
        "w_eu": nrm(ks[25], (DEPTH, N_EXPERTS, D_MODEL, D_EXPERT), D_MODEL ** -0.5),
        "w_ed": nrm(ks[26], (DEPTH, N_EXPERTS, D_EXPERT, D_MODEL), D_EXPERT ** -0.5),
        "g_final": 1.0 + nrm(ks[27], (D_MODEL,), 0.02),
    }


def reference(x_prompt, x_sample, cache_k, cache_v, state_lru_h, state_conv, page_table,
              g_mix, w_in, conv_w, conv_b, w_a, b_a, w_i, b_i, lam, b_sb, g_out_lru, g_out_sb, w_out,
              g_ffn, w_rg, b_rg, w_re, b_re, w_eg, w_eu, w_ed, g_final):
    n_seq, n_pages = page_table.shape
    past_len = n_pages * cache_k.shape[2]
    Bp = x_prompt.shape[0]
    xp, xs = x_prompt, x_sample
    kp_l, vp_l, hp_l, cp_l, ks_l, vs_l, hs_l, cs_l = [], [], [], [], [], [], [], []
    for l in range(DEPTH):
        p = {'g_mix': g_mix[l], 'w_in': w_in[l], 'conv_w': conv_w[l], 'conv_b': conv_b[l],
             'w_a': w_a[l], 'b_a': b_a[l], 'w_i': w_i[l], 'b_i': b_i[l], 'lam': lam[l], 'b_sb': b_sb[l],
             'g_out_lru': g_out_lru[l], 'g_out_sb': g_out_sb[l], 'w_out': w_out[l],
             'g_ffn': g_ffn[l], 'w_rg': w_rg[l], 'b_rg': b_rg[l], 'w_re': w_re[l], 'b_re': b_re[l],
             'w_eg': w_eg[l], 'w_eu': w_eu[l], 'w_ed': w_ed[l]}
        empty = jnp.zeros((Bp, 0, SB_HEADS, HEAD_DIM), xp.dtype)
        xp, kp, vp, hp, cp = layer(xp, jnp.zeros((Bp, LRU_WIDTH), jnp.float32),
                                   jnp.zeros((Bp, CONV_W - 1, LRU_WIDTH), xp.dtype),
                                   empty, empty, 0, p)
        k_past = cache_k[l][page_table].reshape(n_seq, past_len, SB_HEADS, HEAD_DIM)
        v_past = cache_v[l][page_table].reshape(n_seq, past_len, SB_HEADS, HEAD_DIM)
        xs, ksn, vsn, hsn, csn = layer(xs, state_lru_h[l], state_conv[l], k_past, v_past, past_len, p)
        kp_l.append(kp); vp_l.append(vp); hp_l.append(hp); cp_l.append(cp)
        ks_l.append(ksn); vs_l.append(vsn); hs_l.append(hsn); cs_l.append(csn)
    y_prompt = rmsnorm(xp, g_final)
    y_sample = rmsnorm(xs, g_final)
    return (y_prompt, y_sample,
            jnp.stack(kp_l), jnp.stack(vp_l), jnp.stack(hp_l), jnp.stack(cp_l),
            jnp.stack(ks_l), jnp.stack(vs_l), jnp.stack(hs_l), jnp.stack(cs_l))
```

```python
import numpy as np
from contextlib import ExitStack
import concourse.bass as bass
import concourse.mybir as mybir
from concourse.bass_utils import run_bass_kernel_spmd

F32 = mybir.dt.float32; BF16 = mybir.dt.bfloat16; I32 = mybir.dt.int32
AF = mybir.ActivationFunctionType; ALU = mybir.AluOpType

D = 1024; T = 4096; NB = 32; NSB = 8; LW = 512; SBW = 512; NH = 8; HD = 64
INC = 2560; NE = 32; DE = 512; EPS = 1e-6
NS = 4; NST = 16; NPAGES = 64; PAGE = 128
NEG = -30000.0


class _Stop(Exception):
    pass


class Buf:
    __slots__ = ("name", "w", "r")
    def __init__(self, name): self.name = name; self.w = None; self.r = {}


class FW:
    def __init__(self, nc, es):
        self.nc = nc; self.es = es
        self.eng = {"pe": nc.tensor, "act": nc.scalar, "dve": nc.vector, "pool": nc.gpsimd, "sp": nc.sync}
        self.sem = {k: es.enter_context(nc.semaphore("s_" + k)) for k in ("pe", "act", "dve", "pool")}
        self.cnt = {k: 0 for k in self.sem}
        self.seen = {k: {} for k in self.eng}
        self.dsems = [es.enter_context(nc.semaphore("d%d" % i)) for i in range(32)]
        self.dtot = [0] * 32; self.dnext = 0
        self.nins = 0
    def _wait(self, e, ev):
        if ev is None: return
        key, h, v = ev
        if v == 0 or self.seen[e].get(key, 0) >= v: return
        self.eng[e].wait_ge(h, v); self.seen[e][key] = v
    def deps(self, e, reads, writes):
        for b in reads: self._wait(e, b.w)
        for b in writes:
            self._wait(e, b.w)
            for ev in b.r.values(): self._wait(e, ev)
    def _done(self, ev, reads, writes):
        for b in reads: b.r[ev[0]] = ev
        for b in writes: b.w = ev; b.r = {}
    def op(self, e, fns, reads=(), writes=()):
        if not isinstance(fns, (list, tuple)): fns = [fns]
        self.deps(e, reads, writes)
        ins = None
        for fn in fns: ins = fn(); self.nins += 1
        self.cnt[e] += 1
        ins.then_inc(self.sem[e], 1)
        ev = (e, self.sem[e], self.cnt[e])
        self._done(ev, reads, writes)
        return ev
    def dma(self, q, fns, reads=(), writes=()):
        if not isinstance(fns, (list, tuple)): fns = [fns]
        self.deps(q, reads, writes)
        i = self.dnext; self.dnext = (i + 1) % len(self.dsems); h = self.dsems[i]
        self._wait(q, ("d%d" % i, h, self.dtot[i]))
        for fn in fns: fn().then_inc(h, 16); self.nins += 1
        self.dtot[i] += 16 * len(fns)
        ev = ("d%d" % i, h, self.dtot[i])
        self._done(ev, reads, writes)
        return ev
    def barrier(self):
        for e in self.eng:
            for k in self.sem:
                self._wait(e, (k, self.sem[k], self.cnt[k]))
            for i, h in enumerate(self.dsems):
                self._wait(e, ("d%d" % i, h, self.dtot[i]))


def build(cfg=None):
    cfg = cfg or {}
    NPOOL = cfg.get("npool", 2560)
    CAP = cfg.get("cap", 256)
    nsb = cfg.get("nsb", NSB)
    do_moe = cfg.get("moe", True)
    do_sample = cfg.get("sample", True)
    dbg = cfg.get("dbg", False)
    stop = cfg.get("stop", None)
    astage = cfg.get("astage", 4)
    def chk(level):
        if stop is not None and stop == level: raise _Stop()
    nc = bass.Bass("TRN2", target_bir_lowering=False)
    def din(name, shape, dt=F32): return nc.dram_tensor(name, list(shape), dt, kind="ExternalInput").ap()
    def dout(name, shape, dt=F32): return nc.dram_tensor(name, list(shape), dt, kind="ExternalOutput").ap()
    def dscr(name, shape, dt=F32): return nc.dram_tensor(name, list(shape), dt).ap()
    xq = din("xq", [T, D]); valid = din("valid", [128, 1])
    g_mix = din("g_mix", [D]); w_in = din("w_in", [D, INC]); conv_w = din("conv_w", [4, LW]); conv_b = din("conv_b", [LW])
    w_a = din("w_a", [8, 64, 64]); b_a = din("b_a", [LW]); w_i = din("w_i", [8, 64, 64]); b_i = din("b_i", [LW])
    lam = din("lam", [LW]); b_sb = din("b_sb", [NH]); g_out_lru = din("g_out_lru", [LW]); g_out_sb = din("g_out_sb", [SBW])
    w_out = din("w_out", [D, D]); g_ffn = din("g_ffn", [D]); w_rg = din("w_rg", [D, 4]); b_rg = din("b_rg", [4])
    w_re = din("w_re", [D, NE]); b_re = din("b_re", [NE]); g_final = din("g_final", [D])
    w_eg = din("w_eg", [NE, D, DE]); w_eu = din("w_eu", [NE, D, DE]); w_ed = din("w_ed", [NE, DE, D])
    xs = din("xs", [NST, D]); st_h = din("st_h", [NS, LW]); st_conv = din("st_conv", [NS, 3, LW])
    pt = din("pt", [NS, NPAGES], I32); cache_k = din("cache_k", [NPOOL * PAGE, SBW]); cache_v = din("cache_v", [NPOOL * PAGE, SBW])
    y_own = dout("y_own", [T // 2, D]); k_all = dout("k_all", [T, SBW]); v_all = dout("v_all", [T, SBW])
    lru_h = dout("lru_h", [LW]); convb = dout("convb", [3, LW])
    ys = dout("ys", [NST, D]); ks_o = dout("ks", [NST, SBW]); vs_o = dout("vs", [NST, SBW])
    lru_hs = dout("lru_hs", [NS, LW]); convs = dout("convs", [NS, 3, LW])
    if dbg:
        dbg_x1 = dout("dbg_x1", [T // 2, D]); dbg_mix = dout("dbg_mix", [128, 8, T // 2])
    win_bf = dscr("win_bf", [D, INC], BF16); wout_bf = dscr("wout_bf", [D, D], BF16)
    x1_scr = dscr("x1_scr", [T // 2 + 128, D]); bx1scr = Buf("x1scr")
    _NSUP = (2 * (T // 2 + NST)) // 256 + NE
    xs_scr = dscr("xs_scr", [_NSUP * 256 + 128, D], BF16); bxs_w = Buf("xs_scr")
    ys_scr = dscr("ys_scr", [_NSUP * 256 + 128, D]); bys_w = Buf("ys_scr")

    es = ExitStack()
    with es:
        fw = FW(nc, es)
        def sb(name, shape, dt, st=es): return st.enter_context(nc.sbuf_tensor(name, list(shape), dt))
        banks = [es.enter_context(nc.psum_tensor("bank%d" % i, [128, 512], F32)) for i in range(8)]
        bbank = [Buf("bank%d" % i) for i in range(8)]
        out_evs = []

        ident = sb("ident", [128, 128], BF16); b_const = Buf("const")
        m1 = sb("m1", [128, 128], BF16); m2 = sb("m2", [128, 128], BF16)
        ones_bf = sb("ones_bf", [128, 128], BF16)
        rmA = sb("rmA", [128, 512], BF16); rmB = sb("rmB", [128, 512], BF16)
        zer = sb("zer", [2, 128], F32); rmC = sb("rmC", [128, 512], BF16)
        P = nc.gpsimd
        fw.op("pool", lambda: P.memset(zer[:], 0.0), writes=[b_const])
        fw.op("pool", lambda: P.memset(ones_bf[:], 1.0), writes=[b_const])
        fw.op("pool", lambda: P.memset(ident[:], 1.0), writes=[b_const])
        fw.op("pool", lambda: P.affine_select(out=ident[:], in_=ident[:], pattern=[[-1, 128]], compare_op=ALU.is_equal, fill=0.0, base=0, channel_multiplier=1), reads=[b_const], writes=[b_const])
        fw.op("pool", lambda: P.memset(m1[:], 1.0), writes=[b_const])
        fw.op("pool", lambda: P.affine_select(out=m1[:], in_=m1[:], pattern=[[-1, 128]], compare_op=ALU.is_ge, fill=0.0, base=0, channel_multiplier=1), reads=[b_const], writes=[b_const])
        fw.op("pool", lambda: P.memset(m2[:], 1.0), writes=[b_const])
        fw.op("pool", lambda: P.affine_select(out=m2[:], in_=m2[:], pattern=[[1, 128]], compare_op=ALU.is_gt, fill=0.0, base=0, channel_multiplier=-1), reads=[b_const], writes=[b_const])
        fw.op("pool", lambda: P.memset(rmA[:], 0.0), writes=[b_const])
        fw.op("pool", lambda: P.memset(rmB[:], 0.0), writes=[b_const])
        fw.op("pool", lambda: P.memset(rmC[:], 0.0), writes=[b_const])
        for h2 in range(2):
            for rm in (rmB, rmC):
                fw.op("pool", lambda rm=rm, h2=h2: P.memset(rm[:, h2 * 256:h2 * 256 + 128], NEG), reads=[b_const], writes=[b_const])
            for (rm, a) in ((rmA, 0), (rmB, 1)):
                sl = rm[:, h2 * 256 + a * 128: h2 * 256 + a * 128 + 128]
                fw.op("pool", lambda sl=sl: P.affine_select(out=sl, in_=sl, pattern=[[1, 128]], compare_op=ALU.is_gt, fill=NEG, base=0, channel_multiplier=-1), reads=[b_const], writes=[b_const])

        b_par = Buf("params")
        colp = sb("colp", [128, 4, 12], F32)
        def col128(v, c): return v[c * 128:(c + 1) * 128].rearrange("(p o) -> p o", o=1)
        fns = []
        for c in range(4):
            for k in range(4):
                fns.append(lambda c=c, k=k: nc.sync.dma_start(out=colp[:, c, k:k + 1], in_=col128(conv_w[k], c)))
            for i, v in enumerate((conv_b, b_a, b_i, lam, g_out_lru, g_out_sb)):
                fns.append(lambda c=c, i=i, v=v: nc.sync.dma_start(out=colp[:, c, 4 + i:5 + i], in_=col128(v, c)))
        fw.dma("sp", fns, writes=[b_par])
        validt = sb("validt", [128, 1], F32)
        gmix_b = sb("gmix_b", [128, D], F32)
        bsb_row = sb("bsb_row", [2, NH], F32)
        fw.dma("sp", [lambda: nc.sync.dma_start(out=validt[:], in_=valid),
                      lambda: nc.sync.dma_start(out=gmix_b[:], in_=g_mix.partition_broadcast(128)),
                      lambda: nc.sync.dma_start(out=bsb_row[0:1, :], in_=b_sb.rearrange("(o h) -> o h", o=1)),
                      lambda: nc.sync.dma_start(out=bsb_row[1:2, :], in_=b_sb.rearrange("(o h) -> o h", o=1))], writes=[b_par])
        A = nc.scalar; V = nc.vector
        tmpc = sb("tmpc", [128, 4, 1], F32)
        fw.op("act", lambda: A.activation(out=tmpc[:, :, 0], in_=colp[:, :, 7], func=AF.Exp, scale=-1.0), reads=[b_par], writes=[b_par])
        fw.op("act", lambda: A.activation(out=tmpc[:, :, 0], in_=tmpc[:, :, 0], func=AF.Ln, bias=1.0), reads=[b_par], writes=[b_par])
        fw.op("dve", lambda: V.tensor_scalar(out=colp[:, :, 10], in0=tmpc[:, :, 0], scalar1=-8.0, scalar2=None, op0=ALU.mult), reads=[b_par], writes=[b_par])
        fw.op("dve", lambda: V.tensor_scalar(out=colp[:, :, 11], in0=tmpc[:, :, 0], scalar1=-16.0, scalar2=None, op0=ALU.mult), reads=[b_par], writes=[b_par])
        ncol = sb("ncol", [128, 4, 2], F32)
        fw.op("dve", lambda: V.tensor_scalar(out=ncol[:], in0=colp[:, :, 5:7], scalar1=-1.0, scalar2=None, op0=ALU.mult), reads=[b_par], writes=[b_par])
        zrow = sb("zrow", [128, 512], BF16)
        fw.op("dve", lambda: V.memset(zrow[:], 0.0), writes=[b_par])
        brow = sb("brow", [128, 4, 512], BF16); vrow = sb("vrow", [128, 512], BF16)
        bhi = sb("bhi", [2, NH], BF16); bhif = sb("bhif", [2, NH], F32); blo = sb("blo", [2, NH], F32)
        ones2 = sb("ones2", [128, 128], BF16)
        vm1 = sb("vm1", [2, 1], F32)
        fw.op("pool", lambda: P.memset(brow[:], 0.0), writes=[b_par])
        fw.op("pool", lambda: P.memset(vrow[:], 0.0), writes=[b_par])
        fw.op("pool", lambda: P.memset(ones2[:], 0.0), writes=[b_par])
        fw.op("pool", lambda: P.memset(ones2[0:2, :], 1.0), reads=[b_par], writes=[b_par])
        fw.op("dve", lambda: V.tensor_copy(out=bhi[:], in_=bsb_row[:]), reads=[b_par], writes=[b_par])
        fw.op("dve", lambda: V.tensor_copy(out=bhif[:], in_=bhi[:]), reads=[b_par], writes=[b_par])
        fw.op("dve", lambda: V.tensor_sub(out=blo[:], in0=bsb_row[:], in1=bhif[:]), reads=[b_par], writes=[b_par])
        fw.op("dve", lambda: V.tensor_scalar(out=vm1[:], in0=validt[0:2, :], scalar1=-1.0, scalar2=-NEG, op0=ALU.add, op1=ALU.mult), reads=[b_par], writes=[b_par])
        for q in range(4):
            fw.op("dve", lambda q=q: V.tensor_scalar(out=vrow[0:1, q * 128:(q + 1) * 128], in0=zer[0:1, :], scalar1=vm1[0:1, 0:1], scalar2=None, op0=ALU.add), reads=[b_par, b_const], writes=[b_par])
        sel1 = sb("sel1", [2, 1], F32); sel0 = sb("sel0", [2, 1], F32); bcomb = sb("bcomb", [2, NH], F32)
        fw.op("pool", lambda: P.iota(out=sel1[:], pattern=[[0, 1]], base=0, channel_multiplier=1, allow_small_or_imprecise_dtypes=True), writes=[b_par])
        fw.op("dve", lambda: V.tensor_scalar(out=sel0[:], in0=sel1[:], scalar1=-1.0, scalar2=1.0, op0=ALU.mult, op1=ALU.add), reads=[b_par], writes=[b_par])
        fw.op("dve", lambda: V.tensor_scalar(out=bcomb[:], in0=bhif[:], scalar1=sel0[:, 0:1], scalar2=None, op0=ALU.mult), reads=[b_par], writes=[b_par])
        fw.op("dve", lambda: V.scalar_tensor_tensor(out=bcomb[:], in0=blo[:], scalar=sel1[:, 0:1], in1=bcomb[:], op0=ALU.mult, op1=ALU.add), reads=[b_par], writes=[b_par])
        for hp in range(4):
            for h2 in range(2):
                h = 2 * hp + h2
                for a in range(2):
                    cs = slice(h2 * 256 + a * 128, h2 * 256 + a * 128 + 128)
                    fw.op("dve", lambda cs=cs, hp=hp, h=h: V.tensor_scalar(out=brow[0:2, hp, cs], in0=zer[:, :], scalar1=bcomb[:, h:h + 1], scalar2=None, op0=ALU.add), reads=[b_par, b_const], writes=[b_par])
        tmp_es = ExitStack()
        wgate = sb("wgate", [128, 4, 2, 128], BF16); wg_f = sb("wg_f", [128, 4, 2, 128], F32, tmp_es)
        fw.op("pool", lambda: P.memset(wg_f[:], 0.0), writes=[b_par])
        fns = []
        for c in range(4):
            for wi, wsrc in enumerate((w_a, w_i)):
                for b2 in range(2):
                    fns.append(lambda c=c, wi=wi, wsrc=wsrc, b2=b2: nc.sync.dma_start(out=wg_f[b2 * 64:(b2 + 1) * 64, c, wi, b2 * 64:(b2 + 1) * 64], in_=wsrc[2 * c + b2]))
        fw.dma("sp", fns, reads=[], writes=[b_par])
        fw.op("dve", lambda: V.tensor_copy(out=wgate[:], in_=wg_f[:]), reads=[b_par], writes=[b_par])
        b_wscr = Buf("wscr")
        fw.dma("pool", [lambda r=r: P.dma_start(out=win_bf[r * 128:(r + 1) * 128, :], in_=w_in[r * 128:(r + 1) * 128, :], max_dma_last_dim=4096) for r in range(8)]
               + [lambda r=r: P.dma_start(out=wout_bf[r * 128:(r + 1) * 128, :], in_=w_out[r * 128:(r + 1) * 128, :], max_dma_last_dim=4096) for r in range(8)], writes=[b_wscr])

        zt = sb("zt", [128, D], F32, tmp_es)
        fw.op("pool", lambda: P.memset(zt[:], 0.0), writes=[b_par])
        fw.dma("pool", lambda: P.dma_start(out=x1_scr[T // 2:T // 2 + 128, :], in_=zt[:]), reads=[b_par], writes=[bx1scr])
        fw.barrier()
        tmp_es.close()
        ph1 = ExitStack()
        with ph1:
          try:
              chk(0)
              def s1(name, shape, dt): return sb(name, shape, dt, ph1)
              KT = s1("KT", [128, 4, T], BF16); bKT = [Buf("KT%d" % i) for i in range(NB)]
              Vr = s1("Vr", [128, NB, SBW], BF16); bVr = [Buf("Vr%d" % i) for i in range(NB)]
              slab = [s1("slab%d" % i, [128, 8, 1024], BF16) for i in range(2)]; bslab = [Buf("slab%d" % i) for i in range(2)]
              slab_i = [0]
              def load_slab(src_bf, c0, ncol):
                  i = slab_i[0]; slab_i[0] ^= 1
                  fw.dma("sp", lambda: nc.sync.dma_start(out=slab[i][:, :, 0:ncol], in_=src_bf[:, c0:c0 + ncol].rearrange("(kc p) n -> p kc n", p=128)), reads=[b_wscr], writes=[bslab[i]])
                  return slab[i], bslab[i]
              xt = [s1("xt%d" % i, [128, D], F32) for i in range(2)]; bxt = [Buf("xt%d" % i) for i in range(2)]
              hn = [s1("hn%d" % i, [128, D], BF16) for i in range(2)]; bhn = [Buf("hn%d" % i) for i in range(2)]
              stat = s1("stat", [128, 8], F32); bstat = Buf("stat")
              hnT = s1("hnT", [128, 8, 512], BF16); bhnT = Buf("hnT")
              kvst = [s1("kvst%d" % i, [128, 1024], F32) for i in range(2)]; bkvst = [Buf("kvst%d" % i) for i in range(2)]
              kbf = [s1("kbf%d" % i, [128, 512], BF16) for i in range(2)]; bkbf = [Buf("kbf%d" % i) for i in range(2)]
              xlb = s1("xlb", [128, 4, 515], F32); bxlb = [Buf("xlb%d" % c) for c in range(4)]
              ub = s1("ub", [128, 512], F32); bub = Buf("ub"); ubf = s1("ubf", [128, 512], BF16); bubf = Buf("ubf")
              T1 = s1("T1", [128, 512], F32); T2 = s1("T2", [128, 512], F32); T3 = s1("T3", [128, 512], F32)
              bT1 = Buf("T1"); bT2 = Buf("T2"); bT3 = Buf("T3")
              hb = s1("hb", [128, 4, 512], F32); bhb = [Buf("hb%d" % c) for c in range(4)]
              hlast = s1("hlast", [128, 4], F32); bhl = Buf("hlast")
              QT = s1("QT", [128, 4, 2, 256], BF16); bQT = Buf("QT")
              gt = s1("gt", [128, 256], F32); bgt = Buf("gt")
              ylru = s1("ylru", [128, 4, 256], F32); bylru = Buf("ylru")
              ysb = s1("ysb", [128, 4, 256], F32); bysb = Buf("ysb")
              sqb = s1("sqb", [128, 4, 256], BF16); bsqb = Buf("sqb")
              rst = s1("rst", [128, 256], F32); brst = Buf("rst")
              mixT = s1("mixT", [128, 8, 256], BF16); bmix = Buf("mixT")
              x1t = [s1("x1t%d" % i, [128, D], F32) for i in range(1)]; bx1t = [Buf("x1t%d" % i) for i in range(1)]
              NEB = 4
              eb = [s1("eb%d" % i, [128, 512], BF16) for i in range(NEB)]; beb = [Buf("eb%d" % i) for i in range(NEB)]
              lpb = [s1("lpb%d" % i, [128, 512], BF16) for i in range(NEB)]; blpb = [Buf("lpb%d" % i) for i in range(NEB)]
              Eb = [s1("Eb%d" % i, [128, 512], BF16) for i in range(3)]; bEb = [Buf("Eb%d" % i) for i in range(3)]
              wb = [s1("wb%d" % i, [128, 512], BF16) for i in range(3)]; bwb = [Buf("wb%d" % i) for i in range(3)]
              fw.op("dve", lambda: V.memset(hlast[:], 0.0), writes=[bhl])
              fw.op("pool", lambda: P.memset(QT[:], 0.0), writes=[bQT])
              fw.op("dve", lambda: V.memset(xlb[:, :, 0:3], 0.0), writes=bxlb)
              bk_rr = [0]
              def nbank(lo=0, hi=8):
                  i = lo + bk_rr[0] % (hi - lo); bk_rr[0] += 1
                  return i
              PE = nc.tensor
              xt_i = [0]
              for g in range(nsb):
                  for tt in range(4):
                      blk = 4 * g + tt
                      xi = xt_i[0]; xt_i[0] ^= 1
                      fw.dma("sp", lambda xi=xi, blk=blk: nc.sync.dma_start(out=xt[xi][:], in_=xq[blk * 128:(blk + 1) * 128, :]), writes=[bxt[xi]])
                      fw.op("act", lambda xi=xi: A.activation(out=hn[xi][:], in_=xt[xi][:], func=AF.Square, accum_out=stat[:, 0:1]), reads=[bxt[xi]], writes=[bhn[xi], bstat])
                      fw.op("act", lambda: A.activation(out=stat[:, 1:2], in_=stat[:, 0:1], func=AF.Ln, scale=1.0 / D, bias=EPS), reads=[bstat], writes=[bstat])
                      fw.op("act", lambda: A.activation(out=stat[:, 2:3], in_=stat[:, 1:2], func=AF.Exp, scale=-0.5), reads=[bstat], writes=[bstat])
                      fw.op("dve", lambda xi=xi: V.scalar_tensor_tensor(out=hn[xi][:], in0=xt[xi][:], scalar=stat[:, 2:3], in1=gmix_b[:], op0=ALU.mult, op1=ALU.mult), reads=[bxt[xi], bstat, b_par], writes=[bhn[xi]])
                      bi = nbank(0, 2)
                      tb = banks[bi][:].bitcast(BF16)
                      fw.op("pe", [lambda kc=kc, xi=xi, tb=tb: PE.transpose(tb[:, kc * 128:(kc + 1) * 128], hn[xi][:, kc * 128:(kc + 1) * 128], ident[:]) for kc in range(8)], reads=[bhn[xi], b_const], writes=[bbank[bi]])
                      fw.op("act", lambda tb=tb, tt=tt: A.copy(out=hnT[:, :, tt * 128:(tt + 1) * 128], in_=tb.rearrange("p (k t) -> p k t", k=8)), reads=[bbank[bi]], writes=[bhnT])
                  chk(1)
                  wkv, bwkv = load_slab(win_bf, 1536, 1024)
                  for tt in range(4):
                      blk = 4 * g + tt
                      b0 = nbank(2, 8); b1 = nbank(2, 8)
                      for half, bi in ((0, b0), (1, b1)):
                          fw.op("pe", [lambda kc=kc, bi=bi, half=half, tt=tt: PE.matmul(banks[bi][:], hnT[:, kc, tt * 128:(tt + 1) * 128], wkv[:, kc, half * 512:(half + 1) * 512], start=(kc == 0), stop=(kc == 7)) for kc in range(8)], reads=[bhnT, bwkv], writes=[bbank[bi]])
                      si = blk % 2
                      fw.op("act", lambda si=si, b0=b0: A.copy(out=kvst[si][:, 0:512], in_=banks[b0][:]), reads=[bbank[b0]], writes=[bkvst[si]])
                      fw.op("act", lambda si=si, b1=b1: A.copy(out=kvst[si][:, 512:1024], in_=banks[b1][:]), reads=[bbank[b1]], writes=[bkvst[si]])
                      out_evs.append(fw.dma("pool", [lambda si=si, blk=blk: P.dma_start(out=k_all[blk * 128:(blk + 1) * 128, :], in_=kvst[si][:, 0:512]),
                                                     lambda si=si, blk=blk: P.dma_start(out=v_all[blk * 128:(blk + 1) * 128, :], in_=kvst[si][:, 512:1024])], reads=[bkvst[si]]))
                      fw.op("dve", lambda si=si, blk=blk: V.tensor_copy(out=Vr[:, blk, :], in_=kvst[si][:, 512:1024]), reads=[bkvst[si]], writes=[bVr[blk]])
                      fw.op("dve", lambda si=si: V.tensor_copy(out=kbf[si][:], in_=kvst[si][:, 0:512]), reads=[bkvst[si]], writes=[bkbf[si]])
                      bi = nbank(0, 2)
                      tb = banks[bi][:].bitcast(BF16)
                      fw.op("pe", [lambda hp=hp, si=si, tb=tb: PE.transpose(tb[:, hp * 128:(hp + 1) * 128], kbf[si][:, hp * 128:(hp + 1) * 128], ident[:]) for hp in range(4)], reads=[bkbf[si], b_const], writes=[bbank[bi]])
                      fw.op("act", lambda tb=tb, blk=blk: A.copy(out=KT[:, :, blk * 128:(blk + 1) * 128], in_=tb[:, 0:512].rearrange("p (k t) -> p k t", k=4)), reads=[bbank[bi]], writes=[bKT[blk]])
                  chk(2)
                  wxg, bwxg = load_slab(win_bf, 0, 1024)
                  for c in range(4):
                      bi = nbank(2, 8)
                      fw.op("pe", [lambda kc=kc, bi=bi, c=c: PE.matmul(banks[bi][:], wxg[:, kc, c * 128:(c + 1) * 128], hnT[:, kc, :], start=(kc == 0), stop=(kc == 7)) for kc in range(8)], reads=[bhnT, bwxg], writes=[bbank[bi]])
                      fw.op("act", lambda bi=bi, c=c: A.copy(out=xlb[:, c, 3:515], in_=banks[bi][:]), reads=[bbank[bi]], writes=[bxlb[c]])
                      fw.op("dve", lambda c=c: V.tensor_scalar(out=ub[:], in0=xlb[:, c, 0:512], scalar1=colp[:, c, 0:1], scalar2=colp[:, c, 4:5], op0=ALU.mult, op1=ALU.add), reads=[bxlb[c], b_par], writes=[bub])
                      for k in range(1, 4):
                          fw.op("dve", lambda c=c, k=k: V.scalar_tensor_tensor(out=ub[:], in0=xlb[:, c, k:k + 512], scalar=colp[:, c, k:k + 1], in1=ub[:], op0=ALU.mult, op1=ALU.add), reads=[bxlb[c], b_par, bub], writes=[bub])
                      if g == nsb - 1:
                          out_evs.append(fw.dma("pool", [lambda c=c, k=k: P.dma_start(out=convb[k, c * 128:(c + 1) * 128].rearrange("(p o) -> p o", o=1), in_=xlb[:, c, 512 + k:513 + k]) for k in range(3)], reads=[bxlb[c]]))
                      fw.op("pool", lambda c=c: P.tensor_copy(out=ubf[:], in_=ub[:]), reads=[bub], writes=[bubf])
                      ba = nbank(2, 8); bi2 = nbank(2, 8)
                      fw.op("pe", lambda ba=ba, c=c: PE.matmul(banks[ba][:], wgate[:, c, 0, :], ubf[:], start=True, stop=True), reads=[bubf, b_par], writes=[bbank[ba]])
                      fw.op("pe", lambda bi2=bi2, c=c: PE.matmul(banks[bi2][:], wgate[:, c, 1, :], ubf[:], start=True, stop=True), reads=[bubf, b_par], writes=[bbank[bi2]])
                      fw.op("act", lambda ba=ba, c=c: A.activation(out=T1[:], in_=banks[ba][:], func=AF.Exp, scale=-1.0, bias=ncol[:, c, 0:1]), reads=[bbank[ba], b_par], writes=[bT1])
                      fw.op("act", lambda: A.activation(out=T1[:], in_=T1[:], func=AF.Ln, bias=1.0), reads=[bT1], writes=[bT1])
                      fw.op("act", lambda: A.activation(out=T1[:], in_=T1[:], func=AF.Exp, scale=-1.0), reads=[bT1], writes=[bT1])
                      fw.op("act", lambda bi2=bi2, c=c: A.activation(out=T3[:], in_=banks[bi2][:], func=AF.Exp, scale=-1.0, bias=ncol[:, c, 1:2]), reads=[bbank[bi2], b_par], writes=[bT3])
                      fw.op("act", lambda: A.activation(out=T3[:], in_=T3[:], func=AF.Ln, bias=1.0), reads=[bT3], writes=[bT3])
                      fw.op("act", lambda: A.activation(out=T3[:], in_=T3[:], func=AF.Exp, scale=-1.0), reads=[bT3], writes=[bT3])
                      fw.op("act", lambda c=c: A.activation(out=T2[:], in_=T1[:], func=AF.Exp, scale=colp[:, c, 10:11]), reads=[bT1, b_par], writes=[bT2])
                      fw.op("act", lambda c=c: A.activation(out=T1[:], in_=T1[:], func=AF.Exp, scale=colp[:, c, 11:12]), reads=[bT1, b_par], writes=[bT1])
                      fw.op("dve", lambda: V.tensor_scalar(out=T1[:], in0=T1[:], scalar1=1.0 - 1e-7, scalar2=-1.0, op0=ALU.min, op1=ALU.mult), reads=[bT1], writes=[bT1])
                      fw.op("act", lambda: A.activation(out=T1[:], in_=T1[:], func=AF.Ln, bias=1.0), reads=[bT1], writes=[bT1])
                      fw.op("act", lambda: A.activation(out=T1[:], in_=T1[:], func=AF.Exp, scale=0.5), reads=[bT1], writes=[bT1])
                      fw.op("dve", lambda: V.tensor_mul(out=T3[:], in0=T3[:], in1=ub[:]), reads=[bT3, bub], writes=[bT3])
                      fw.op("dve", lambda: V.tensor_mul(out=T3[:], in0=T3[:], in1=T1[:]), reads=[bT3, bT1], writes=[bT3])
                      if g == 0:
                          fw.op("dve", lambda: V.tensor_scalar(out=T3[:, 0:128], in0=T3[:, 0:128], scalar1=validt[:, 0:1], scalar2=None, op0=ALU.mult), reads=[bT3, b_par], writes=[bT3])
                      fw.op("dve", lambda c=c: V.tensor_tensor_scan(out=hb[:, c, :], data0=T2[:], data1=T3[:], initial=hlast[:, c:c + 1], op0=ALU.mult, op1=ALU.add), reads=[bT2, bT3, bhl], writes=[bhb[c]])
                      fw.op("dve", lambda c=c: V.tensor_copy(out=hlast[:, c:c + 1], in_=hb[:, c, 511:512]), reads=[bhb[c]], writes=[bhl])
                      fw.op("dve", lambda c=c: V.tensor_copy(out=xlb[:, c, 0:3], in_=xlb[:, c, 512:515]), reads=[bxlb[c]], writes=[bxlb[c]])
                      if g == nsb - 1:
                          out_evs.append(fw.dma("pool", lambda c=c: P.dma_start(out=lru_h[c * 128:(c + 1) * 128].rearrange("(p o) -> p o", o=1), in_=hb[:, c, 511:512]), reads=[bhb[c]]))
                  chk(3)
                  def own(ap2d):
                      return ap2d.rearrange("p (q t) -> p q t", t=128)[:, 1::2, :]
                  for c in range(4):
                      bi = nbank(2, 8)
                      po = banks[bi][:, 0:256]
                      fw.op("pe", [lambda kc=kc, bi=bi, c=c: PE.matmul(banks[bi][:, 0:256].rearrange("p (q t) -> p q t", t=128), wxg[:, kc, 512 + c * 128:512 + (c + 1) * 128], own(hnT[:, kc, :]), start=(kc == 0), stop=(kc == 7)) for kc in range(8)], reads=[bhnT, bwxg], writes=[bbank[bi]])
                      fw.op("act", lambda po=po: A.activation(out=gt[:], in_=po, func=AF.Square), reads=[bbank[bi]], writes=[bgt])
                      fw.op("dve", lambda: V.tensor_scalar(out=gt[:], in0=gt[:], scalar1=0.0713548163, scalar2=1.5957691216, op0=ALU.mult, op1=ALU.add), reads=[bgt], writes=[bgt])
                      fw.op("dve", lambda po=po: V.tensor_mul(out=gt[:], in0=gt[:], in1=po), reads=[bgt, bbank[bi]], writes=[bgt])
                      fw.op("act", lambda: A.activation(out=gt[:], in_=gt[:], func=AF.Exp, scale=-1.0), reads=[bgt], writes=[bgt])
                      fw.op("act", lambda: A.activation(out=gt[:], in_=gt[:], func=AF.Ln, bias=1.0), reads=[bgt], writes=[bgt])
                      fw.op("act", lambda: A.activation(out=gt[:], in_=gt[:], func=AF.Exp, scale=-1.0), reads=[bgt], writes=[bgt])
                      fw.op("dve", lambda po=po: V.tensor_mul(out=gt[:], in0=gt[:], in1=po), reads=[bgt, bbank[bi]], writes=[bgt])
                      fw.op("dve", lambda c=c: V.tensor_mul(out=ylru[:, c, :].rearrange("p (q t) -> p q t", t=128), in0=gt[:].rearrange("p (q t) -> p q t", t=128), in1=own(hb[:, c, :])), reads=[bgt, bhb[c]], writes=[bylru])
                  chk(4)
                  wq, bwq = load_slab(win_bf, 1024, 512)
                  for hp in range(4):
                      bi = nbank(2, 8)
                      fw.op("pe", [lambda kc=kc, bi=bi, hp=hp: PE.matmul(banks[bi][:, 0:256].rearrange("p (q t) -> p q t", t=128), wq[:, kc, hp * 128:(hp + 1) * 128], own(hnT[:, kc, :]), start=(kc == 0), stop=(kc == 7)) for kc in range(8)], reads=[bhnT, bwq], writes=[bbank[bi]])
                      for h2 in range(2):
                          fw.op("act", lambda bi=bi, hp=hp, h2=h2: A.activation(out=QT[h2 * 64:(h2 + 1) * 64, hp, h2, :], in_=banks[bi][h2 * 64:(h2 + 1) * 64, 0:256], func=AF.Copy, scale=0.125), reads=[bbank[bi]], writes=[bQT])
                  chk(5)
                  kbmax = 4 * g + 3
                  items = [(kb, hp) for kb in range(kbmax, -1, -1) for hp in range(4)]
                  n = len(items)
                  Sb = [0, 1]; Pb = [2, 3, 4, 5]; Yb = [6, 7]
                  def yview(hp):
                      return banks[Yb[hp // 2]][:, (hp % 2) * 256:(hp % 2) * 256 + 256]
                  for yb in Yb:
                      fw.op("pe", lambda yb=yb: PE.matmul(banks[yb][:], ones2[:], zrow[:], start=True, stop=False), reads=[b_par], writes=[bbank[yb]])
                  for i in range(n + 3):
                      if i < n:
                          kb, hp = items[i]
                          sbk = Sb[i % 2]; ei = i % NEB
                          fl = [lambda sbk=sbk, hp=hp: PE.matmul(banks[sbk][:], ones2[:], brow[:, hp, :], start=True, stop=False)]
                          if kb == 0:
                              fl.append(lambda sbk=sbk: PE.matmul(banks[sbk][:], ones2[:], vrow[:], start=False, stop=False))
                          rm = {4 * g + 1: rmA, 4 * g + 2: rmC, 4 * g + 3: rmB}.get(kb)
                          if rm is not None:
                              fl.append(lambda sbk=sbk, rm=rm: PE.matmul(banks[sbk][:], ident[:], rm[:], start=False, stop=False))
                          for h2 in range(2):
                              fl.append(lambda sbk=sbk, h2=h2, hp=hp, kb=kb: PE.matmul(banks[sbk][:, h2 * 256:(h2 + 1) * 256], KT[:, hp, kb * 128:(kb + 1) * 128], QT[:, hp, h2, :], start=False, stop=(h2 == 1)))
                          fw.op("pe", fl, reads=[bKT[kb], bQT, b_par, b_const], writes=[bbank[sbk]])
                          fw.op("act", lambda sbk=sbk, ei=ei: A.activation(out=eb[ei][:], in_=banks[sbk][:], func=AF.Exp), reads=[bbank[sbk]], writes=[beb[ei]])
                          fw.op("act", lambda ei=ei: A.activation(out=lpb[ei][:], in_=eb[ei][:], func=AF.Ln, bias=1.0), reads=[beb[ei]], writes=[blpb[ei]])
                      i1 = i - 1
                      if 0 <= i1 < n and astage >= 2:
                          kb, hp = items[i1]; ei = i1 % NEB; pb = Pb[hp]; Ei = i1 % 3
                          fw.op("pe", lambda pb=pb, ei=ei, kb=kb: PE.matmul(banks[pb][:], m1[:], lpb[ei][:], start=(kb == kbmax), stop=False), reads=[blpb[ei], b_const], writes=[bbank[pb]])
                          fw.op("act", lambda pb=pb, Ei=Ei: A.activation(out=Eb[Ei][:], in_=banks[pb][:], func=AF.Exp, scale=-1.0), reads=[bbank[pb]], writes=[bEb[Ei]])
                      i2 = i - 2
                      if 0 <= i2 < n and astage >= 3:
                          kb, hp = items[i2]; ei = i2 % NEB; pb = Pb[hp]; Ei = i2 % 3; wi = i2 % 3
                          if kb > 0:
                              fw.op("pe", lambda pb=pb, ei=ei: PE.matmul(banks[pb][:], m2[:], lpb[ei][:], start=False, stop=False), reads=[blpb[ei], b_const], writes=[bbank[pb]])
                          fw.op("dve", lambda ei=ei, Ei=Ei, wi=wi: V.tensor_mul(out=wb[wi][:], in0=eb[ei][:], in1=Eb[Ei][:]), reads=[beb[ei], bEb[Ei]], writes=[bwb[wi]])
                      i3 = i - 3
                      if 0 <= i3 < n and astage >= 4:
                          kb, hp = items[i3]; wi = i3 % 3; yb = Yb[hp // 2]
                          yv = yview(hp)
                          fw.op("pe", [lambda h2=h2, wi=wi, kb=kb, hp=hp, yv=yv: PE.matmul(yv[h2 * 64:(h2 + 1) * 64, :], Vr[:, kb, (2 * hp + h2) * 64:(2 * hp + h2 + 1) * 64], wb[wi][:, h2 * 256:(h2 + 1) * 256], start=False, stop=(kb == 0)) for h2 in range(2)], reads=[bwb[wi], bVr[kb]], writes=[bbank[yb]])
                  for hp in range(4):
                      fw.op("act", lambda hp=hp: A.copy(out=ysb[:, hp, :], in_=yview(hp)), reads=[bbank[Yb[hp // 2]]], writes=[bysb])
                  chk(6)
                  for (ysrc, bsrc, gcol, off) in ((ylru, bylru, 8, 0), (ysb, bysb, 9, 4)):
                      fw.op("act", lambda ysrc=ysrc: A.activation(out=sqb[:], in_=ysrc[:], func=AF.Square), reads=[bsrc], writes=[bsqb])
                      bi = nbank(2, 6)
                      fw.op("pe", [lambda c=c, bi=bi: PE.matmul(banks[bi][:, 0:256], ones_bf[:], sqb[:, c, :], start=(c == 0), stop=(c == 3)) for c in range(4)], reads=[bsqb, b_const], writes=[bbank[bi]])
                      fw.op("act", lambda bi=bi: A.activation(out=rst[:], in_=banks[bi][:, 0:256], func=AF.Ln, scale=1.0 / 512, bias=EPS), reads=[bbank[bi]], writes=[brst])
                      fw.op("act", lambda: A.activation(out=rst[:], in_=rst[:], func=AF.Exp, scale=-0.5), reads=[brst], writes=[brst])
                      for c in range(4):
                          fw.op("dve", lambda c=c, ysrc=ysrc, gcol=gcol, off=off: V.scalar_tensor_tensor(out=mixT[:, off + c, :], in0=ysrc[:, c, :], scalar=colp[:, c, gcol:gcol + 1], in1=rst[:], op0=ALU.mult, op1=ALU.mult), reads=[bsrc, brst, b_par], writes=[bmix])
                  if dbg:
                      out_evs.append(fw.dma("pool", lambda g=g: P.dma_start(out=dbg_mix[:, :, g * 256:(g + 1) * 256], in_=mixT[:]), reads=[bmix]))
                  chk(7)
                  wo, bwo = load_slab(wout_bf, 0, 1024)
                  for a in range(2):
                      blk = 4 * g + 1 + 2 * a; oi = 2 * g + a
                      xi = xt_i[0]; xt_i[0] ^= 1
                      fw.dma("sp", lambda xi=xi, blk=blk: nc.sync.dma_start(out=xt[xi][:], in_=xq[blk * 128:(blk + 1) * 128, :]), writes=[bxt[xi]])
                      b0 = nbank(2, 6); b1 = nbank(2, 6)
                      for half, bi in ((0, b0), (1, b1)):
                          fw.op("pe", [lambda kc=kc, bi=bi, half=half, a=a: PE.matmul(banks[bi][:], mixT[:, kc, a * 128:(a + 1) * 128], wo[:, kc, half * 512:(half + 1) * 512], start=(kc == 0), stop=(kc == 7)) for kc in range(8)], reads=[bmix, bwo], writes=[bbank[bi]])
                      xo = 0
                      for half, bi in ((0, b0), (1, b1)):
                          fw.op("dve", lambda xo=xo, xi=xi, half=half, bi=bi: V.tensor_add(out=x1t[xo][:, half * 512:(half + 1) * 512], in0=banks[bi][:], in1=xt[xi][:, half * 512:(half + 1) * 512]), reads=[bbank[bi], bxt[xi]], writes=[bx1t[xo]])
                      fw.dma("pool", lambda xo=xo, oi=oi: P.dma_start(out=x1_scr[oi * 128:(oi + 1) * 128, :], in_=x1t[xo][:]), reads=[bx1t[xo]], writes=[bx1scr])
                      if dbg:
                          out_evs.append(fw.dma("pool", lambda xo=xo, oi=oi: P.dma_start(out=dbg_x1[oi * 128:(oi + 1) * 128, :], in_=x1t[xo][:]), reads=[bx1t[xo]]))
          except _Stop:
              pass
          fw.barrier()
        if do_sample:
          ph2 = ExitStack()
          with ph2:
            def s2(name, shape, dt): return sb(name, shape, dt, ph2)
            PE = nc.tensor
            NQ = NST
            b_s = Buf("s_par")
            idF = s2("idF", [128, 128], F32)
            fw.op("pool", lambda: P.memset(idF[:], 1.0), writes=[b_s])
            fw.op("pool", lambda: P.affine_select(out=idF[:], in_=idF[:], pattern=[[-1, 128]], compare_op=ALU.is_equal, fill=0.0, base=0, channel_multiplier=1), reads=[b_s], writes=[b_s])
            slb = s2("slb", [128, 8, 1024], BF16); bslb = Buf("slb")
            xs_t = s2("xs_t", [NQ, D], F32); hn_s = s2("hn_s", [NQ, D], BF16); bxs_t = Buf("xs_t"); bhn_s = Buf("hn_s")
            sts = s2("sts", [NQ, 8], F32); bsts = Buf("sts")
            hnT_s = s2("hnT_s", [128, 8, NQ], BF16); bhnT_s = Buf("hnT_s")
            gmix16 = gmix_b
            fw.dma("sp", lambda: nc.sync.dma_start(out=xs_t[:], in_=xs), writes=[bxs_t])
            fw.op("act", lambda: A.activation(out=hn_s[:], in_=xs_t[:], func=AF.Square, accum_out=sts[:, 0:1]), reads=[bxs_t], writes=[bhn_s, bsts])
            fw.op("act", lambda: A.activation(out=sts[:, 1:2], in_=sts[:, 0:1], func=AF.Ln, scale=1.0 / D, bias=EPS), reads=[bsts], writes=[bsts])
            fw.op("act", lambda: A.activation(out=sts[:, 2:3], in_=sts[:, 1:2], func=AF.Exp, scale=-0.5), reads=[bsts], writes=[bsts])
            fw.op("dve", lambda: V.scalar_tensor_tensor(out=hn_s[:], in0=xs_t[:], scalar=sts[:, 2:3], in1=gmix_b[0:NQ, :], op0=ALU.mult, op1=ALU.mult), reads=[bxs_t, bsts, b_par], writes=[bhn_s])
            tb = banks[0][:].bitcast(BF16)
            fw.op("pe", [lambda kc=kc: PE.transpose(tb[:, kc * NQ:(kc + 1) * NQ], hn_s[:, kc * 128:(kc + 1) * 128], ident[0:NQ, 0:NQ]) for kc in range(8)], reads=[bhn_s, b_const], writes=[bbank[0]])
            fw.op("act", lambda: A.copy(out=hnT_s[:], in_=tb[:, 0:8 * NQ].rearrange("p (k t) -> p k t", k=8)), reads=[bbank[0]], writes=[bhnT_s])
            pj = s2("pj", [NQ, INC], F32); bpj = Buf("pj")
            for grp in range(3):
                c0 = (0, 1024, 2048)[grp]; ncl = (1024, 1024, 512)[grp]
                fw.dma("sp", lambda c0=c0, ncl=ncl: nc.sync.dma_start(out=slb[:, :, 0:ncl], in_=win_bf[:, c0:c0 + ncl].rearrange("(kc p) n -> p kc n", p=128)), reads=[b_wscr], writes=[bslb])
                for hf in range(ncl // 512):
                    bi = 2 + hf
                    fw.op("pe", [lambda kc=kc, bi=bi, hf=hf: PE.matmul(banks[bi][0:NQ, :], hnT_s[:, kc, :], slb[:, kc, hf * 512:(hf + 1) * 512], start=(kc == 0), stop=(kc == 7)) for kc in range(8)], reads=[bhnT_s, bslb], writes=[bbank[bi]])
                    fw.op("act", lambda bi=bi, c0=c0, hf=hf: A.copy(out=pj[:, c0 + hf * 512:c0 + (hf + 1) * 512], in_=banks[bi][0:NQ, :]), reads=[bbank[bi]], writes=[bpj])
            out_evs.append(fw.dma("sp", [lambda: nc.sync.dma_start(out=ks_o[:, :], in_=pj[:, 1536:2048]), lambda: nc.sync.dma_start(out=vs_o[:, :], in_=pj[:, 2048:2560])], reads=[bpj]))
            qkb = s2("qkb", [NQ, 1024], BF16); vnb = s2("vnb", [NQ, SBW], BF16); bqkb = Buf("qkb")
            fw.op("dve", lambda: V.tensor_copy(out=qkb[:], in_=pj[:, 1024:2048]), reads=[bpj], writes=[bqkb])
            fw.op("dve", lambda: V.tensor_copy(out=vnb[:], in_=pj[:, 2048:2560]), reads=[bpj], writes=[bqkb])
            xgT = s2("xgT", [128, 8, NQ], F32); bxgT = Buf("xgT")
            fw.op("pe", [lambda c=c: PE.transpose(banks[1][:, c * NQ:(c + 1) * NQ], pj[:, c * 128:(c + 1) * 128], idF[0:NQ, 0:NQ]) for c in range(8)], reads=[bpj, b_s], writes=[bbank[1]])
            fw.op("act", lambda: A.copy(out=xgT[:], in_=banks[1][:, 0:8 * NQ].rearrange("p (k t) -> p k t", k=8)), reads=[bbank[1]], writes=[bxgT])
            QTs = s2("QTs", [128, 4, 2, NQ], BF16); KTn = s2("KTn", [128, 4, NQ], BF16); bQTs = Buf("QTs")
            fw.op("pool", lambda: P.memset(QTs[:], 0.0), writes=[bQTs])
            tb = banks[0][:].bitcast(BF16)
            fw.op("pe", [lambda c=c: PE.transpose(tb[:, c * NQ:(c + 1) * NQ], qkb[:, c * 128:(c + 1) * 128], ident[0:NQ, 0:NQ]) for c in range(8)], reads=[bqkb, b_const], writes=[bbank[0]])
            for hp in range(4):
                for h2 in range(2):
                    fw.op("act", lambda hp=hp, h2=h2, tb=tb: A.activation(out=QTs[h2 * 64:(h2 + 1) * 64, hp, h2, :], in_=tb[h2 * 64:(h2 + 1) * 64, hp * NQ:(hp + 1) * NQ], func=AF.Copy, scale=0.125), reads=[bbank[0]], writes=[bQTs])
            fw.op("act", lambda tb=tb: A.copy(out=KTn[:], in_=tb[:, 4 * NQ:8 * NQ].rearrange("p (k t) -> p k t", k=4)), reads=[bbank[0]], writes=[bQTs])
            stc = s2("stc", [16, LW], F32); bstc = Buf("stc")
            fw.dma("sp", [lambda: nc.sync.dma_start(out=stc[0:12, :], in_=st_conv.rearrange("s k c -> (s k) c")), lambda: nc.sync.dma_start(out=stc[12:16, :], in_=st_h)], writes=[bstc])
            stT = s2("stT", [128, 4, 16], F32); bstT = Buf("stT")
            fw.op("pe", [lambda c=c: PE.transpose(banks[1][:, c * 16:(c + 1) * 16], stc[:, c * 128:(c + 1) * 128], idF[0:16, 0:16]) for c in range(4)], reads=[bstc, b_s], writes=[bbank[1]])
            fw.op("act", lambda: A.copy(out=stT[:], in_=banks[1][:, 0:64].rearrange("p (c t) -> p c t", c=4)), reads=[bbank[1]], writes=[bstT])
            xls = s2("xls", [128, 4, NS, 7], F32); bxls = Buf("xls")
            fw.op("dve", lambda: V.tensor_copy(out=xls[:, :, :, 0:3], in_=stT[:, :, 0:12].rearrange("p c (s k) -> p c s k", s=NS)), reads=[bstT], writes=[bxls])
            fw.op("dve", lambda: V.tensor_copy(out=xls[:, :, :, 3:7], in_=xgT[:, 0:4, :].rearrange("p c (s t) -> p c s t", s=NS)), reads=[bxgT], writes=[bxls])
            us = s2("us", [128, 4, NQ], F32); usb = s2("usb", [128, 4, NQ], BF16); bus = Buf("us")
            L1 = s2("L1", [128, 4, NQ], F32); L2 = s2("L2", [128, 4, NQ], F32); L3 = s2("L3", [128, 4, NQ], F32); bL = Buf("L")
            hs_s = s2("hs_s", [128, 4, NQ], F32); bhs = Buf("hs_s")
            for c in range(4):
                uv = us[:, c, :].rearrange("p (s t) -> p s t", s=NS)
                fw.op("dve", lambda c=c, uv=uv: V.tensor_scalar(out=uv, in0=xls[:, c, :, 0:4], scalar1=colp[:, c, 0:1], scalar2=colp[:, c, 4:5], op0=ALU.mult, op1=ALU.add), reads=[bxls, b_par], writes=[bus])
                for k in range(1, 4):
                    fw.op("dve", lambda c=c, k=k, uv=uv: V.scalar_tensor_tensor(out=uv, in0=xls[:, c, :, k:k + 4], scalar=colp[:, c, k:k + 1], in1=uv, op0=ALU.mult, op1=ALU.add), reads=[bxls, b_par, bus], writes=[bus])
            fw.op("dve", lambda: V.tensor_copy(out=usb[:], in_=us[:]), reads=[bus], writes=[bus])
            for c in range(4):
                fw.op("pe", lambda c=c: PE.matmul(banks[2][:, c * NQ:(c + 1) * NQ], wgate[:, c, 0, :], usb[:, c, :], start=True, stop=True), reads=[bus, b_par], writes=[bbank[2]])
                fw.op("pe", lambda c=c: PE.matmul(banks[3][:, c * NQ:(c + 1) * NQ], wgate[:, c, 1, :], usb[:, c, :], start=True, stop=True), reads=[bus, b_par], writes=[bbank[3]])
            for c in range(4):
                fw.op("act", lambda c=c: A.activation(out=L1[:, c, :], in_=banks[2][:, c * NQ:(c + 1) * NQ], func=AF.Exp, scale=-1.0, bias=ncol[:, c, 0:1]), reads=[bbank[2], b_par], writes=[bL])
                fw.op("act", lambda c=c: A.activation(out=L3[:, c, :], in_=banks[3][:, c * NQ:(c + 1) * NQ], func=AF.Exp, scale=-1.0, bias=ncol[:, c, 1:2]), reads=[bbank[3], b_par], writes=[bL])
            for Lx in (L1, L3):
                fw.op("act", lambda Lx=Lx: A.activation(out=Lx[:], in_=Lx[:], func=AF.Ln, bias=1.0), reads=[bL], writes=[bL])
                fw.op("act", lambda Lx=Lx: A.activation(out=Lx[:], in_=Lx[:], func=AF.Exp, scale=-1.0), reads=[bL], writes=[bL])
            for c in range(4):
                fw.op("act", lambda c=c: A.activation(out=L2[:, c, :], in_=L1[:, c, :], func=AF.Exp, scale=colp[:, c, 10:11]), reads=[bL, b_par], writes=[bL])
                fw.op("act", lambda c=c: A.activation(out=L1[:, c, :], in_=L1[:, c, :], func=AF.Exp, scale=colp[:, c, 11:12]), reads=[bL, b_par], writes=[bL])
            fw.op("dve", lambda: V.tensor_scalar(out=L1[:], in0=L1[:], scalar1=1.0 - 1e-7, scalar2=-1.0, op0=ALU.min, op1=ALU.mult), reads=[bL], writes=[bL])
            fw.op("act", lambda: A.activation(out=L1[:], in_=L1[:], func=AF.Ln, bias=1.0), reads=[bL], writes=[bL])
            fw.op("act", lambda: A.activation(out=L1[:], in_=L1[:], func=AF.Exp, scale=0.5), reads=[bL], writes=[bL])
            fw.op("dve", lambda: V.tensor_mul(out=L3[:], in0=L3[:], in1=us[:]), reads=[bL, bus], writes=[bL])
            fw.op("dve", lambda: V.tensor_mul(out=L3[:], in0=L3[:], in1=L1[:]), reads=[bL], writes=[bL])
            for c in range(4):
                for sq in range(NS):
                    fw.op("dve", lambda c=c, sq=sq: V.tensor_tensor_scan(out=hs_s[:, c, sq * 4:(sq + 1) * 4], data0=L2[:, c, sq * 4:(sq + 1) * 4], data1=L3[:, c, sq * 4:(sq + 1) * 4], initial=stT[:, c, 12 + sq:13 + sq], op0=ALU.mult, op1=ALU.add), reads=[bL, bstT], writes=[bhs])
            fin = s2("fin", [128, 4, 16], F32); bfin = Buf("fin")
            fw.op("dve", lambda: V.tensor_copy(out=fin[:, :, 0:12].rearrange("p c (s k) -> p c s k", s=NS), in_=xls[:, :, :, 4:7]), reads=[bxls], writes=[bfin])
            fw.op("dve", lambda: V.tensor_copy(out=fin[:, :, 12:16], in_=hs_s[:].rearrange("p c (s t) -> p c s t", s=NS)[:, :, :, 3]), reads=[bhs], writes=[bfin])
            finT = s2("finT", [16, LW], F32); bfinT = Buf("finT")
            fw.op("pe", [lambda c=c: PE.transpose(banks[1][0:16, c * 128:(c + 1) * 128], fin[:, c, :], idF[:]) for c in range(4)], reads=[bfin, b_s], writes=[bbank[1]])
            fw.op("act", lambda: A.copy(out=finT[:], in_=banks[1][0:16, :]), reads=[bbank[1]], writes=[bfinT])
            out_evs.append(fw.dma("sp", [lambda: nc.sync.dma_start(out=convs.rearrange("s k c -> (s k) c"), in_=finT[0:12, :]), lambda: nc.sync.dma_start(out=lru_hs[:, :], in_=finT[12:16, :])], reads=[bfinT]))
            gsm = s2("gsm", [128, 4, NQ], F32); bgsm = Buf("gsm"); yl_s = s2("yl_s", [128, 4, NQ], F32); ysb_s = s2("ysb_s", [128, 4, NQ], F32); byl = Buf("yl_s")
            GL = xgT[:, 4:8, :]
            fw.op("act", lambda: A.activation(out=gsm[:], in_=GL, func=AF.Square), reads=[bxgT], writes=[bgsm])
            fw.op("dve", lambda: V.tensor_scalar(out=gsm[:], in0=gsm[:], scalar1=0.0713548163, scalar2=1.5957691216, op0=ALU.mult, op1=ALU.add), reads=[bgsm], writes=[bgsm])
            fw.op("dve", lambda: V.tensor_mul(out=gsm[:], in0=gsm[:], in1=GL), reads=[bgsm, bxgT], writes=[bgsm])
            fw.op("act", lambda: A.activation(out=gsm[:], in_=gsm[:], func=AF.Exp, scale=-1.0), reads=[bgsm], writes=[bgsm])
            fw.op("act", lambda: A.activation(out=gsm[:], in_=gsm[:], func=AF.Ln, bias=1.0), reads=[bgsm], writes=[bgsm])
            fw.op("act", lambda: A.activation(out=gsm[:], in_=gsm[:], func=AF.Exp, scale=-1.0), reads=[bgsm], writes=[bgsm])
            fw.op("dve", lambda: V.tensor_mul(out=gsm[:], in0=gsm[:], in1=GL), reads=[bgsm, bxgT], writes=[bgsm])
            fw.op("dve", lambda: V.tensor_mul(out=yl_s[:], in0=gsm[:], in1=hs_s[:]), reads=[bgsm, bhs], writes=[byl])
            NCOL = 32
            brs = s2("brs", [128, NCOL], BF16); mkn = s2("mkn", [16, NS, NCOL], BF16); mkf = s2("mkf", [16, NS, NCOL], F32)
            fw.op("pool", lambda: P.memset(brs[:], 0.0), writes=[b_s])
            for h in range(NH):
                fw.op("dve", lambda h=h: V.tensor_scalar(out=brs[0:2, h * 4:(h + 1) * 4], in0=zer[:, 0:4], scalar1=bcomb[:, h:h + 1], scalar2=None, op0=ALU.add), reads=[b_s, b_par, b_const], writes=[b_s])
            fw.op("pool", lambda: P.memset(mkf[:], 0.0), writes=[b_s])
            for sq in range(NS):
                fw.op("pool", lambda sq=sq: P.affine_select(out=mkf[:, sq, :], in_=mkf[:, sq, :], pattern=[[0, NCOL]], compare_op=ALU.is_ge, fill=NEG, base=-4 * sq, channel_multiplier=1), reads=[b_s], writes=[b_s])
                fw.op("pool", lambda sq=sq: P.affine_select(out=mkf[:, sq, :].rearrange("p (h t) -> p h t", t=4), in_=mkf[:, sq, :].rearrange("p (h t) -> p h t", t=4), pattern=[[0, 8], [1, 4]], compare_op=ALU.is_gt, fill=NEG, base=4 * sq, channel_multiplier=-1), reads=[b_s], writes=[b_s])
            fw.op("dve", lambda: V.tensor_copy(out=mkn[:], in_=mkf[:]), reads=[b_s], writes=[b_s])
            pti = s2("pti", [128, NS * NPAGES], I32); ptf = s2("ptf", [128, NS * NPAGES], F32); pidx = s2("pidx", [128, NS * NPAGES], I32); iop = s2("iop", [128, 1], F32); bpidx = Buf("pidx")
            fw.dma("sp", lambda: nc.sync.dma_start(out=pti[:], in_=pt.rearrange("s g -> (s g)").partition_broadcast(128)), writes=[bpidx])
            fw.op("pool", lambda: P.iota(out=iop[:], pattern=[[0, 1]], base=0, channel_multiplier=1, allow_small_or_imprecise_dtypes=True), writes=[b_s])
            fw.op("dve", lambda: V.tensor_copy(out=ptf[:], in_=pti[:]), reads=[bpidx], writes=[bpidx])
            fw.op("dve", lambda: V.tensor_scalar(out=ptf[:], in0=ptf[:], scalar1=float(PAGE), scalar2=iop[:, 0:1], op0=ALU.mult, op1=ALU.add), reads=[bpidx, b_s], writes=[bpidx])
            fw.op("dve", lambda: V.tensor_copy(out=pidx[:], in_=ptf[:]), reads=[bpidx], writes=[bpidx])
            NPB = 4
            kpg = [s2("kpg%d" % i, [128, SBW], BF16) for i in range(NPB)]; vpg = [s2("vpg%d" % i, [128, SBW], BF16) for i in range(NPB)]
            bkpg = [Buf("kpg%d" % i) for i in range(NPB)]; bvpg = [Buf("vpg%d" % i) for i in range(NPB)]
            ktp = [s2("ktp%d" % i, [128, 4, 128], BF16) for i in range(2)]; bktp = [Buf("ktp%d" % i) for i in range(2)]
            NSB_ = 4
            es_ = [s2("es%d" % i, [128, NCOL], BF16) for i in range(NSB_)]; ls_ = [s2("ls%d" % i, [128, NCOL], BF16) for i in range(NSB_)]
            Es_ = [s2("Es%d" % i, [128, NCOL], BF16) for i in range(NSB_)]; ws_ = [s2("ws%d" % i, [128, NCOL], BF16) for i in range(NSB_)]
            bes = [Buf("es%d" % i) for i in range(NSB_)]; bls = [Buf("ls%d" % i) for i in range(NSB_)]; bEs = [Buf("Es%d" % i) for i in range(NSB_)]; bws = [Buf("ws%d" % i) for i in range(NSB_)]
            fw.op("pe", lambda: PE.matmul(banks[4][:, 0:NS * NCOL], ones2[:], zrow[:, 0:NS * NCOL], start=True, stop=False), reads=[b_par], writes=[bbank[4]])
            fw.op("pe", lambda: PE.matmul(banks[6][:, 0:64], ones2[:], zrow[:, 0:64], start=True, stop=False), reads=[b_par], writes=[bbank[6]])
            cnt_i = [0]
            def attend_block(sq, M, kt_ap, kt_bufs, v_ap, v_bufs, first, own):
                i = cnt_i[0]; cnt_i[0] += 1
                si = i % NSB_; sbk = 2 + (i % 2); c0 = ((i // 2) % 4) * NCOL
                S = banks[sbk][0:M, c0:c0 + NCOL]
                fl = [lambda: PE.matmul(S, ones2[:, 0:M], brs[:], start=True, stop=False)]
                if own:
                    fl.append(lambda: PE.matmul(S, ident[0:16, 0:16], mkn[:, sq, :], start=False, stop=False))
                for hp in range(4):
                    for h2 in range(2):
                        cc = (hp * 2 + h2) * 4
                        fl.append(lambda hp=hp, h2=h2, cc=cc: PE.matmul(banks[sbk][0:M, c0 + cc:c0 + cc + 4], kt_ap(hp), QTs[:, hp, h2, sq * 4:(sq + 1) * 4], start=False, stop=(hp == 3 and h2 == 1)))
                fw.op("pe", fl, reads=kt_bufs + [bQTs, b_s, b_par, b_const], writes=[bbank[sbk]])
                fw.op("act", lambda: A.activation(out=es_[si][0:M, :], in_=S, func=AF.Exp), reads=[bbank[sbk]], writes=[bes[si]])
                fw.op("act", lambda: A.activation(out=ls_[si][0:M, :], in_=es_[si][0:M, :], func=AF.Ln, bias=1.0), reads=[bes[si]], writes=[bls[si]])
                Pq = banks[4][:, sq * NCOL:(sq + 1) * NCOL]
                if own:
                    Po = banks[5][0:M, sq * NCOL:(sq + 1) * NCOL]
                    fw.op("pe", lambda: PE.matmul(Po, m1[0:M, 0:M], ls_[si][0:M, :], start=True, stop=True), reads=[bls[si], b_const], writes=[bbank[5]])
                    fw.op("act", lambda: A.activation(out=Es_[si][0:M, :], in_=Po, func=AF.Exp, scale=-1.0), reads=[bbank[5]], writes=[bEs[si]])
                    fw.op("pe", lambda: PE.matmul(Pq, ones_bf[0:M, :], ls_[si][0:M, :], start=False, stop=False), reads=[bls[si], b_const], writes=[bbank[4]])
                else:
                    fw.op("pe", lambda: PE.matmul(Pq, m1[:], ls_[si][:], start=False, stop=False), reads=[bls[si], b_const], writes=[bbank[4]])
                    fw.op("act", lambda: A.activation(out=Es_[si][:], in_=Pq, func=AF.Exp, scale=-1.0), reads=[bbank[4]], writes=[bEs[si]])
                    fw.op("pe", lambda: PE.matmul(Pq, m2[:], ls_[si][:], start=False, stop=False), reads=[bls[si], b_const], writes=[bbank[4]])
                fw.op("dve", lambda: V.tensor_mul(out=ws_[si][0:M, :], in0=es_[si][0:M, :], in1=Es_[si][0:M, :]), reads=[bes[si], bEs[si]], writes=[bws[si]])
                fl = []
                for hp in range(4):
                    for h2 in range(2):
                        cc = (hp * 2 + h2) * 4
                        fl.append(lambda hp=hp, h2=h2, cc=cc: PE.matmul(banks[6][h2 * 64:(h2 + 1) * 64, hp * NQ + sq * 4:hp * NQ + sq * 4 + 4], v_ap(hp * 2 + h2), ws_[si][0:M, cc:cc + 4], start=False, stop=False))
                fw.op("pe", fl, reads=v_bufs + [bws[si]], writes=[bbank[6]])
            for sq in range(NS):
                attend_block(sq, NQ, lambda hp: KTn[:, hp, :], [bQTs], lambda h: vnb[:, h * 64:(h + 1) * 64], [bqkb], True, True)
            npg = cfg.get("npages", NPAGES)
            for pg in range(npg - 1, -1, -1):
                for sq in range(NS):
                    j = cnt_i[0]; bi_ = j % NPB; ki = j % 2
                    col = sq * NPAGES + pg
                    fw.dma("pool", [lambda bi_=bi_, col=col: P.indirect_dma_start(out=kpg[bi_][:], out_offset=None, in_=cache_k, in_offset=bass.IndirectOffsetOnAxis(ap=pidx[:, col:col + 1], axis=0)),
                                    lambda bi_=bi_, col=col: P.indirect_dma_start(out=vpg[bi_][:], out_offset=None, in_=cache_v, in_offset=bass.IndirectOffsetOnAxis(ap=pidx[:, col:col + 1], axis=0))], reads=[bpidx], writes=[bkpg[bi_], bvpg[bi_]])
                    tb = banks[0][:].bitcast(BF16) if ki == 0 else banks[1][:].bitcast(BF16)
                    bt = 0 if ki == 0 else 1
                    fw.op("pe", [lambda hp=hp, tb=tb, bi_=bi_: PE.transpose(tb[:, hp * 128:(hp + 1) * 128], kpg[bi_][:, hp * 128:(hp + 1) * 128], ident[:]) for hp in range(4)], reads=[bkpg[bi_], b_const], writes=[bbank[bt]])
                    fw.op("dve", lambda tb=tb, ki=ki: V.tensor_copy(out=ktp[ki][:], in_=tb[:, 0:512].rearrange("p (k t) -> p k t", k=4)), reads=[bbank[bt]], writes=[bktp[ki]])
                    attend_block(sq, 128, lambda hp, ki=ki: ktp[ki][:, hp, :], [bktp[ki]], lambda h, bi_=bi_: vpg[bi_][:, h * 64:(h + 1) * 64], [bvpg[bi_]], False, False)
            fw.op("act", lambda: A.copy(out=ysb_s[:], in_=banks[6][:, 0:64].rearrange("p (c t) -> p c t", c=4)), reads=[bbank[6]], writes=[byl])
            sq_s = s2("sq_s", [128, 4, NQ], BF16); rs_s = s2("rs_s", [128, NQ], F32); mixT_s = s2("mixT_s", [128, 8, NQ], BF16); bmx = Buf("mixT_s")
            for (ysrc, gcol, off) in ((yl_s, 8, 0), (ysb_s, 9, 4)):
                fw.op("act", lambda ysrc=ysrc: A.activation(out=sq_s[:], in_=ysrc[:], func=AF.Square), reads=[byl], writes=[bmx])
                fw.op("pe", [lambda c=c: PE.matmul(banks[7][:, 0:NQ], ones_bf[:], sq_s[:, c, :], start=(c == 0), stop=(c == 3)) for c in range(4)], reads=[bmx, b_const], writes=[bbank[7]])
                fw.op("act", lambda: A.activation(out=rs_s[:], in_=banks[7][:, 0:NQ], func=AF.Ln, scale=1.0 / 512, bias=EPS), reads=[bbank[7]], writes=[bmx])
                fw.op("act", lambda: A.activation(out=rs_s[:], in_=rs_s[:], func=AF.Exp, scale=-0.5), reads=[bmx], writes=[bmx])
                for c in range(4):
                    fw.op("dve", lambda c=c, ysrc=ysrc, gcol=gcol, off=off: V.scalar_tensor_tensor(out=mixT_s[:, off + c, :], in0=ysrc[:, c, :], scalar=colp[:, c, gcol:gcol + 1], in1=rs_s[:], op0=ALU.mult, op1=ALU.mult), reads=[byl, bmx, b_par], writes=[bmx])
            fw.dma("sp", lambda: nc.sync.dma_start(out=slb[:, :, :], in_=wout_bf[:, :].rearrange("(kc p) n -> p kc n", p=128)), reads=[b_wscr], writes=[bslb])
            x1s = s2("x1s", [NQ, D], F32); bx1s = Buf("x1s")
            for hf in range(2):
                bi = 2 + hf
                fw.op("pe", [lambda kc=kc, bi=bi, hf=hf: PE.matmul(banks[bi][0:NQ, :], mixT_s[:, kc, :], slb[:, kc, hf * 512:(hf + 1) * 512], start=(kc == 0), stop=(kc == 7)) for kc in range(8)], reads=[bmx, bslb], writes=[bbank[bi]])
                fw.op("dve", lambda bi=bi, hf=hf: V.tensor_add(out=x1s[:, hf * 512:(hf + 1) * 512], in0=banks[bi][0:NQ, :], in1=xs_t[:, hf * 512:(hf + 1) * 512]), reads=[bbank[bi], bxs_t], writes=[bx1s])
            fw.dma("pool", lambda: P.dma_start(out=x1_scr[T // 2:T // 2 + NQ, :], in_=x1s[:]), reads=[bx1s], writes=[bx1scr])
            if dbg:
                dbg_x1s = dout("dbg_x1s", [NQ, D]); dbg_mixs = dout("dbg_mixs", [128, 8, NQ], BF16)
                out_evs.append(fw.dma("pool", [lambda: P.dma_start(out=dbg_x1s[:, :], in_=x1s[:]), lambda: P.dma_start(out=dbg_mixs[:, :, :], in_=mixT_s[:])], reads=[bx1s, bmx]))
            fw.barrier()
        NT3 = 17
        SUP = 256
        NSUP = (2 * (T // 2 + NST)) // SUP + NE
        DUMMY = NSUP * SUP
        if do_moe:
          ph3 = ExitStack()
          with ph3:
            def s3(name, shape, dt): return sb(name, shape, dt, ph3)
            b_p3 = Buf("p3par")
            gffn_b = s3("gffn_b", [128, D], F32); gfin_b = s3("gfin_b", [128, D], F32)
            wr = s3("wr", [128, 8, 36], F32); rbias = s3("rbias", [128, 36], F32)
            identF = s3("identF", [128, 128], F32)
            tvalid = s3("tvalid", [128, 1], F32); iot = s3("iot", [128, 1], F32)
            OHst = s3("OHst", [128, NT3, 2, NE], F32); bOHst = Buf("OHst")
            h2all = s3("h2all", [128, NT3, D], BF16); bh2all = [Buf("h2all%d" % i) for i in range(NT3)]
            carry = s3("carry", [128, NE], F32); bcarry = Buf("carry")
            meta = s3("meta", [128, NT3, 4], F32); bmeta = Buf("meta")
            sloti = s3("sloti", [128, NT3, 2], I32)
            fw.dma("sp", [lambda: nc.sync.dma_start(out=gffn_b[:], in_=g_ffn.partition_broadcast(128)),
                          lambda: nc.sync.dma_start(out=gfin_b[:], in_=g_final.partition_broadcast(128)),
                          lambda: nc.sync.dma_start(out=wr[:, :, 0:4], in_=w_rg.rearrange("(kc p) n -> p kc n", p=128)),
                          lambda: nc.sync.dma_start(out=wr[:, :, 4:36], in_=w_re.rearrange("(kc p) n -> p kc n", p=128)),
                          lambda: nc.sync.dma_start(out=rbias[:, 0:4], in_=b_rg.partition_broadcast(128)),
                          lambda: nc.sync.dma_start(out=rbias[:, 4:36], in_=b_re.partition_broadcast(128))], writes=[b_p3])
            fw.op("pool", lambda: P.memset(identF[:], 1.0), writes=[b_p3])
            fw.op("pool", lambda: P.affine_select(out=identF[:], in_=identF[:], pattern=[[-1, 128]], compare_op=ALU.is_equal, fill=0.0, base=0, channel_multiplier=1), reads=[b_p3], writes=[b_p3])
            fw.op("pool", lambda: P.iota(out=iot[:], pattern=[[0, 1]], base=0, channel_multiplier=1, allow_small_or_imprecise_dtypes=True), writes=[b_p3])
            fw.op("dve", lambda: V.tensor_scalar(out=tvalid[:], in0=iot[:], scalar1=float(NST) - 0.5, scalar2=None, op0=ALU.is_lt), reads=[b_p3], writes=[b_p3])
            fw.op("dve", lambda: V.memset(carry[:], 0.0), writes=[bcarry])
            x3 = [s3("x3_%d" % i, [128, D], F32) for i in range(2)]; bx3 = [Buf("x3_%d" % i) for i in range(2)]
            h2f = s3("h2f", [128, D], F32); bh2f = Buf("h2f")
            h2T = s3("h2T", [128, 8, 128], F32); bh2T = Buf("h2T")
            st3 = s3("st3", [128, 8], F32); bst3 = Buf("st3")
            rt = s3("rt", [128, 256], F32); brt = Buf("rt")
            ohb = s3("ohb", [128, NE], BF16); bohb = Buf("ohb")
            for t in range(NT3):
                xi = t % 2
                fw.dma("sp", lambda xi=xi, t=t: nc.sync.dma_start(out=x3[xi][:], in_=x1_scr[t * 128:(t + 1) * 128, :]), reads=[bx1scr], writes=[bx3[xi]])
                fw.op("act", lambda xi=xi: A.activation(out=h2f[:], in_=x3[xi][:], func=AF.Square, accum_out=st3[:, 0:1]), reads=[bx3[xi]], writes=[bh2f, bst3])
                fw.op("act", lambda: A.activation(out=st3[:, 1:2], in_=st3[:, 0:1], func=AF.Ln, scale=1.0 / D, bias=EPS), reads=[bst3], writes=[bst3])
                fw.op("act", lambda: A.activation(out=st3[:, 2:3], in_=st3[:, 1:2], func=AF.Exp, scale=-0.5), reads=[bst3], writes=[bst3])
                fw.op("dve", lambda xi=xi: V.scalar_tensor_tensor(out=h2f[:], in0=x3[xi][:], scalar=st3[:, 2:3], in1=gffn_b[:], op0=ALU.mult, op1=ALU.mult), reads=[bx3[xi], bst3, b_p3], writes=[bh2f])
                fw.op("pool", lambda t=t: P.tensor_copy(out=h2all[:, t, :], in_=h2f[:]), reads=[bh2f], writes=[bh2all[t]])
                for half in range(2):
                    bi = nbank3 = 2 + half
                    fw.op("pe", [lambda kc=kc, bi=bi: PE.transpose(banks[bi][:, (kc % 4) * 128:(kc % 4 + 1) * 128], h2f[:, kc * 128:(kc + 1) * 128], identF[:]) for kc in range(4 * half, 4 * half + 4)], reads=[bh2f, b_p3], writes=[bbank[bi]])
                    fw.op("act", lambda bi=bi, half=half: A.copy(out=h2T[:, 4 * half:4 * half + 4, :], in_=banks[bi][:].rearrange("p (k t) -> p k t", k=4)), reads=[bbank[bi]], writes=[bh2T])
                bl = 4
                fw.op("pe", [lambda kc=kc: PE.matmul(banks[bl][:, 0:36], h2T[:, kc, :], wr[:, kc, :], start=(kc == 0), stop=(kc == 7)) for kc in range(8)], reads=[bh2T, b_p3], writes=[bbank[bl]])
                LG = rt[:, 0:36]; GOH = rt[:, 36:40]; TMP = rt[:, 40:72]; IG = rt[:, 72:80]; OH1e = rt[:, 80:88]; IG2 = rt[:, 88:96]; OH2e = rt[:, 96:104]
                OH1 = rt[:, 104:136]; OH2 = rt[:, 136:168]; RK = rt[:, 168:200]
                def sc(i): return rt[:, 200 + i:201 + i]
                rw = [brt]
                fw.op("dve", lambda: V.tensor_add(out=LG, in0=banks[bl][:, 0:36], in1=rbias[:]), reads=[bbank[bl], b_p3], writes=rw)
                fw.op("dve", lambda: V.reduce_max(out=sc(0), in_=rt[:, 0:4], axis=mybir.AxisListType.X), reads=rw, writes=rw)
                fw.op("dve", lambda: V.tensor_scalar(out=GOH, in0=rt[:, 0:4], scalar1=sc(0), scalar2=None, op0=ALU.is_ge), reads=rw, writes=rw)
                fw.op("dve", lambda: V.tensor_scalar(out=sc(1), in0=sc(0), scalar1=-1.0, scalar2=None, op0=ALU.mult), reads=rw, writes=rw)
                fw.op("act", lambda: A.activation(out=rt[:, 216:220], in_=rt[:, 0:4], func=AF.Exp, bias=sc(1), accum_out=sc(2)), reads=rw, writes=rw)
                fw.op("dve", lambda: V.reciprocal(out=sc(3), in_=sc(2)), reads=rw, writes=rw)
                fw.op("dve", lambda: V.tensor_mul(out=TMP.rearrange("p (g e) -> p g e", g=4), in0=rt[:, 4:36].rearrange("p (g e) -> p g e", g=4), in1=GOH.unsqueeze(2).to_broadcast([128, 4, 8])), reads=rw, writes=rw)
                fw.op("dve", lambda: V.reduce_sum(out=IG, in_=TMP.rearrange("p (g e) -> p e g", g=4), axis=mybir.AxisListType.X), reads=rw, writes=rw)
                fw.op("dve", lambda: V.reduce_max(out=sc(4), in_=IG, axis=mybir.AxisListType.X), reads=rw, writes=rw)
                fw.op("dve", lambda: V.tensor_scalar(out=OH1e, in0=IG, scalar1=sc(4), scalar2=None, op0=ALU.is_ge), reads=rw, writes=rw)
                fw.op("dve", lambda: V.scalar_tensor_tensor(out=IG2, in0=OH1e, scalar=-1e30, in1=IG, op0=ALU.mult, op1=ALU.add), reads=rw, writes=rw)
                fw.op("dve", lambda: V.reduce_max(out=sc(5), in_=IG2, axis=mybir.AxisListType.X), reads=rw, writes=rw)
                fw.op("dve", lambda: V.tensor_scalar(out=OH2e, in0=IG2, scalar1=sc(5), scalar2=None, op0=ALU.is_ge), reads=rw, writes=rw)
                fw.op("dve", lambda: V.tensor_sub(out=sc(6), in0=sc(5), in1=sc(4)), reads=rw, writes=rw)
                fw.op("act", lambda: A.activation(out=sc(6), in_=sc(6), func=AF.Exp), reads=rw, writes=rw)
                fw.op("dve", lambda: V.tensor_scalar(out=sc(6), in0=sc(6), scalar1=1.0, scalar2=None, op0=ALU.add), reads=rw, writes=rw)
                fw.op("dve", lambda: V.reciprocal(out=sc(7), in_=sc(6)), reads=rw, writes=rw)
                fw.op("dve", lambda: V.tensor_scalar(out=sc(8), in0=sc(7), scalar1=-1.0, scalar2=1.0, op0=ALU.mult, op1=ALU.add), reads=rw, writes=rw)
                fw.op("dve", lambda t=t: V.tensor_mul(out=meta[:, t, 2:3], in0=sc(7), in1=sc(3)), reads=rw, writes=[bmeta])
                fw.op("dve", lambda t=t: V.tensor_mul(out=meta[:, t, 3:4], in0=sc(8), in1=sc(3)), reads=rw, writes=[bmeta])
                for (OHk, ohe) in ((OH1, OH1e), (OH2, OH2e)):
                    fw.op("dve", lambda OHk=OHk, ohe=ohe: V.tensor_mul(out=OHk.rearrange("p (g e) -> p g e", g=4), in0=GOH.unsqueeze(2).to_broadcast([128, 4, 8]), in1=ohe.unsqueeze(1).to_broadcast([128, 4, 8])), reads=rw, writes=rw)
                    if t == NT3 - 1:
                        fw.op("dve", lambda OHk=OHk: V.tensor_scalar(out=OHk, in0=OHk, scalar1=tvalid[:, 0:1], scalar2=None, op0=ALU.mult), reads=rw + [b_p3], writes=rw)
                fw.op("dve", lambda: V.tensor_add(out=ohb[:], in0=OH1, in1=OH2), reads=rw, writes=[bohb])
                br = 5; bc = 6
                fw.op("pe", lambda: PE.matmul(banks[br][:, 0:NE], m2[:], ohb[:], start=True, stop=True), reads=[bohb, b_const], writes=[bbank[br]])
                fw.op("pe", lambda: PE.matmul(banks[bc][:, 0:NE], ones_bf[:], ohb[:], start=True, stop=True), reads=[bohb, b_const], writes=[bbank[bc]])
                fw.op("dve", lambda: V.tensor_add(out=RK, in0=banks[br][:, 0:NE], in1=carry[:]), reads=[bbank[br], bcarry], writes=rw)
                fw.op("dve", lambda: V.tensor_add(out=carry[:], in0=carry[:], in1=banks[bc][:, 0:NE]), reads=[bbank[bc], bcarry], writes=[bcarry])
                for k, OHk in ((0, OH1), (1, OH2)):
                    fw.op("dve", lambda OHk=OHk: V.tensor_mul(out=TMP, in0=OHk, in1=RK), reads=rw, writes=rw)
                    fw.op("dve", lambda t=t, k=k: V.reduce_sum(out=meta[:, t, k:k + 1], in_=TMP, axis=mybir.AxisListType.X), reads=rw, writes=[bmeta])
                    fw.op("dve", lambda t=t, k=k, OHk=OHk: V.tensor_copy(out=OHst[:, t, k, :], in_=OHk), reads=rw, writes=[bOHst])
            lay = s3("lay", [128, 640], F32); blay = Buf("lay")
            NTH = 10
            thr = lay[:, 0:NTH]; ntl = lay[:, 16:48]; incl = lay[:, 48:80]; base = lay[:, 80:112]; onesl = lay[:, 112:144]
            iti = lay[:, 144:144 + NSUP]; ei = lay[:, 192:192 + NSUP]; rowoff = lay[:, 240:248]
            cmp1 = s3("cmp1", [128, NE * NSUP], F32)
            widx_f = s3("widx_f", [128, NSUP, 12], F32); widx = s3("widx", [128, NSUP, 12], I32); bwidx = Buf("widx")
            lw = [blay]
            fw.op("pool", lambda: P.iota(out=thr, pattern=[[SUP, NTH]], base=0, channel_multiplier=0, allow_small_or_imprecise_dtypes=True), writes=lw)
            fw.op("pool", lambda: P.iota(out=iti, pattern=[[1, NSUP]], base=0, channel_multiplier=0, allow_small_or_imprecise_dtypes=True), reads=lw, writes=lw)
            fw.op("pool", lambda: P.iota(out=rowoff, pattern=[[128, 8]], base=0, channel_multiplier=1, allow_small_or_imprecise_dtypes=True), reads=lw, writes=lw)
            fw.op("pool", lambda: P.memset(onesl, 1.0), reads=lw, writes=lw)
            fw.op("dve", lambda: V.tensor_tensor(out=cmp1[:, 0:NE * NTH].rearrange("p (e j) -> p e j", e=NE), in0=carry[:].unsqueeze(2).to_broadcast([128, NE, NTH]), in1=thr.unsqueeze(1).to_broadcast([128, NE, NTH]), op=ALU.is_gt), reads=[bcarry] + lw, writes=lw)
            fw.op("dve", lambda: V.reduce_sum(out=ntl, in_=cmp1[:, 0:NE * NTH].rearrange("p (e j) -> p e j", e=NE), axis=mybir.AxisListType.X), reads=lw, writes=lw)
            fw.op("dve", lambda: V.tensor_tensor_scan(out=incl, data0=onesl, data1=ntl, initial=0.0, op0=ALU.mult, op1=ALU.add), reads=lw, writes=lw)
            fw.op("dve", lambda: V.tensor_sub(out=base, in0=incl, in1=ntl), reads=lw, writes=lw)
            fw.op("dve", lambda: V.tensor_scalar(out=base, in0=base, scalar1=float(SUP), scalar2=None, op0=ALU.mult), reads=lw, writes=lw)
            fw.op("dve", lambda: V.tensor_tensor(out=cmp1[:].rearrange("p (i e) -> p i e", i=NSUP), in0=incl.unsqueeze(1).to_broadcast([128, NSUP, NE]), in1=iti.unsqueeze(2).to_broadcast([128, NSUP, NE]), op=ALU.is_le), reads=lw, writes=lw)
            fw.op("dve", lambda: V.reduce_sum(out=ei, in_=cmp1[:].rearrange("p (i e) -> p i e", i=NSUP), axis=mybir.AxisListType.X), reads=lw, writes=lw)
            fw.op("dve", lambda: V.tensor_scalar(out=ei, in0=ei, scalar1=float(NE - 1), scalar2=None, op0=ALU.min), reads=lw, writes=lw)
            fw.op("dve", lambda: V.scalar_tensor_tensor(out=widx_f[:, :, 0:8], in0=ei.unsqueeze(2).to_broadcast([128, NSUP, 8]), scalar=float(D), in1=rowoff.unsqueeze(1).to_broadcast([128, NSUP, 8]), op0=ALU.mult, op1=ALU.add), reads=lw, writes=[bwidx])
            fw.op("dve", lambda: V.scalar_tensor_tensor(out=widx_f[:, :, 8:12], in0=ei.unsqueeze(2).to_broadcast([128, NSUP, 4]), scalar=float(DE), in1=rowoff[:, 0:4].unsqueeze(1).to_broadcast([128, NSUP, 4]), op0=ALU.mult, op1=ALU.add), reads=lw, writes=[bwidx])
            fw.op("dve", lambda: V.tensor_copy(out=widx[:], in_=widx_f[:]), reads=[bwidx], writes=[bwidx])
            for t in range(NT3):
                for k in range(2):
                    fw.op("dve", lambda t=t, k=k: V.tensor_mul(out=rt[:, 40:72], in0=OHst[:, t, k, :], in1=base), reads=[bOHst] + lw, writes=[brt])
                    fw.op("dve", lambda: V.reduce_sum(out=rt[:, 200:201], in_=rt[:, 40:72], axis=mybir.AxisListType.X), reads=[brt], writes=[brt])
                    fw.op("dve", lambda t=t, k=k: V.tensor_add(out=meta[:, t, k:k + 1], in0=meta[:, t, k:k + 1], in1=rt[:, 200:201]), reads=[brt, bmeta], writes=[bmeta])
                    if t == NT3 - 1:
                        fw.op("dve", lambda t=t, k=k: V.tensor_scalar(out=meta[:, t, k:k + 1], in0=meta[:, t, k:k + 1], scalar1=-float(DUMMY), scalar2=tvalid[:, 0:1], op0=ALU.add, op1=ALU.mult), reads=[bmeta, b_p3], writes=[bmeta])
                        fw.op("dve", lambda t=t, k=k: V.tensor_scalar(out=meta[:, t, k:k + 1], in0=meta[:, t, k:k + 1], scalar1=float(DUMMY), scalar2=None, op0=ALU.add), reads=[bmeta], writes=[bmeta])
                    fw.op("dve", lambda t=t, k=k: V.tensor_copy(out=sloti[:, t, k:k + 1], in_=meta[:, t, k:k + 1]), reads=[bmeta], writes=[bmeta])
                    fw.dma("pool", lambda t=t, k=k: P.indirect_dma_start(out=xs_scr, out_offset=bass.IndirectOffsetOnAxis(ap=sloti[:, t, k:k + 1], axis=0), in_=h2all[:, t, :], in_offset=None), reads=[bmeta, bh2all[t]], writes=[bxs_w])
            wgb = [s3("wgb%d" % i, [128, 8, DE], BF16) for i in range(2)]; wub = [s3("wub%d" % i, [128, 8, DE], BF16) for i in range(2)]
            wdb = [s3("wdb%d" % i, [128, 4, D], BF16) for i in range(2)]; bwe = [Buf("we%d" % i) for i in range(2)]
            NTE = SUP // 128
            CAP = SUP
            weg_rows = w_eg.rearrange("e r n -> (e r) n"); weu_rows = w_eu.rearrange("e r n -> (e r) n"); wed_rows = w_ed.rearrange("e r n -> (e r) n")
            xe = [s3("xe%d" % i, [128, NTE, D], BF16) for i in range(2)]; bxe = [Buf("xe%d" % i) for i in range(2)]
            xeT = s3("xeT", [128, 8, CAP], BF16); bxeT = Buf("xeT")
            sg = s3("sg", [128, CAP], F32); bsg = Buf("sg")
            aT = s3("aT", [128, 4, CAP], BF16); baT = Buf("aT")
            yt3 = [s3("yt3_%d" % i, [128, D], F32) for i in range(2)]; byt3 = [Buf("yt3_%d" % i) for i in range(2)]
            for e in range(NSUP):
                wi = e % 2
                fl = []
                for kc in range(8):
                    fl.append(lambda e=e, wi=wi, kc=kc: P.indirect_dma_start(out=wgb[wi][:, kc, :], out_offset=None, in_=weg_rows, in_offset=bass.IndirectOffsetOnAxis(ap=widx[:, e, kc:kc + 1], axis=0)))
                    fl.append(lambda e=e, wi=wi, kc=kc: P.indirect_dma_start(out=wub[wi][:, kc, :], out_offset=None, in_=weu_rows, in_offset=bass.IndirectOffsetOnAxis(ap=widx[:, e, kc:kc + 1], axis=0)))
                for kc in range(4):
                    fl.append(lambda e=e, wi=wi, kc=kc: P.indirect_dma_start(out=wdb[wi][:, kc, :], out_offset=None, in_=wed_rows, in_offset=bass.IndirectOffsetOnAxis(ap=widx[:, e, 8 + kc:9 + kc], axis=0)))
                fw.dma("pool", fl, reads=[bwidx], writes=[bwe[wi]])
                fw.dma("sp", lambda e=e, wi=wi: nc.sync.dma_start(out=xe[wi][:], in_=xs_scr[e * CAP:(e + 1) * CAP, :].rearrange("(t p) n -> p t n", p=128)), reads=[bxs_w], writes=[bxe[wi]])
                for tt in range(NTE):
                    bi = tt % 2
                    tb = banks[bi][:].bitcast(BF16)
                    fw.op("pe", [lambda kc=kc, tb=tb, wi=wi, tt=tt: PE.transpose(tb[:, kc * 128:(kc + 1) * 128], xe[wi][:, tt, kc * 128:(kc + 1) * 128], ident[:]) for kc in range(8)], reads=[bxe[wi], b_const], writes=[bbank[bi]])
                    fw.op("dve", lambda tb=tb, tt=tt: V.tensor_copy(out=xeT[:, :, tt * 128:(tt + 1) * 128], in_=tb.rearrange("p (k t) -> p k t", k=8)), reads=[bbank[bi]], writes=[bxeT])
                for m in range(4):
                    bg = 2 + (m % 2) * 2; bu = bg + 1
                    fw.op("pe", [lambda kc=kc, bg=bg, m=m, wi=wi: PE.matmul(banks[bg][:, 0:CAP], wgb[wi][:, kc, m * 128:(m + 1) * 128], xeT[:, kc, :], start=(kc == 0), stop=(kc == 7)) for kc in range(8)], reads=[bwe[wi], bxeT], writes=[bbank[bg]])
                    fw.op("pe", [lambda kc=kc, bu=bu, m=m, wi=wi: PE.matmul(banks[bu][:, 0:CAP], wub[wi][:, kc, m * 128:(m + 1) * 128], xeT[:, kc, :], start=(kc == 0), stop=(kc == 7)) for kc in range(8)], reads=[bwe[wi], bxeT], writes=[bbank[bu]])
                    fw.op("act", lambda bg=bg: A.activation(out=sg[:], in_=banks[bg][:, 0:CAP], func=AF.Sigmoid), reads=[bbank[bg]], writes=[bsg])
                    fw.op("dve", lambda bg=bg: V.tensor_mul(out=sg[:], in0=sg[:], in1=banks[bg][:, 0:CAP]), reads=[bsg, bbank[bg]], writes=[bsg])
                    fw.op("dve", lambda bu=bu, m=m: V.tensor_mul(out=aT[:, m, :], in0=sg[:], in1=banks[bu][:, 0:CAP]), reads=[bsg, bbank[bu]], writes=[baT])
                for tt in range(NTE):
                    yi = tt % 2
                    for half in range(2):
                        bi = 6 + half
                        fw.op("pe", [lambda kc=kc, bi=bi, half=half, tt=tt, wi=wi: PE.matmul(banks[bi][:], aT[:, kc, tt * 128:(tt + 1) * 128], wdb[wi][:, kc, half * 512:(half + 1) * 512], start=(kc == 0), stop=(kc == 3)) for kc in range(4)], reads=[baT, bwe[wi]], writes=[bbank[bi]])
                        fw.op("act", lambda bi=bi, half=half, yi=yi: A.copy(out=yt3[yi][:, half * 512:(half + 1) * 512], in_=banks[bi][:]), reads=[bbank[bi]], writes=[byt3[yi]])
                    fw.dma("sp", lambda e=e, tt=tt, yi=yi: nc.sync.dma_start(out=ys_scr[e * CAP + tt * 128:e * CAP + (tt + 1) * 128, :], in_=yt3[yi][:]), reads=[byt3[yi]], writes=[bys_w])
            r1 = [s3("r1_%d" % i, [128, D], F32) for i in range(2)]; r2 = [s3("r2_%d" % i, [128, D], F32) for i in range(2)]
            br12 = [Buf("r12_%d" % i) for i in range(2)]
            for t in range(NT3):
                xi = t % 2
                fw.dma("sp", lambda xi=xi, t=t: nc.sync.dma_start(out=x3[xi][:], in_=x1_scr[t * 128:(t + 1) * 128, :]), reads=[bx1scr], writes=[bx3[xi]])
                fw.dma("pool", [lambda xi=xi, t=t: P.indirect_dma_start(out=r1[xi][:], out_offset=None, in_=ys_scr, in_offset=bass.IndirectOffsetOnAxis(ap=sloti[:, t, 0:1], axis=0)),
                                lambda xi=xi, t=t: P.indirect_dma_start(out=r2[xi][:], out_offset=None, in_=ys_scr, in_offset=bass.IndirectOffsetOnAxis(ap=sloti[:, t, 1:2], axis=0))], reads=[bys_w, bmeta], writes=[br12[xi]])
                fw.op("dve", lambda xi=xi, t=t: V.scalar_tensor_tensor(out=x3[xi][:], in0=r1[xi][:], scalar=meta[:, t, 2:3], in1=x3[xi][:], op0=ALU.mult, op1=ALU.add), reads=[br12[xi], bmeta, bx3[xi]], writes=[bx3[xi]])
                fw.op("dve", lambda xi=xi, t=t: V.scalar_tensor_tensor(out=x3[xi][:], in0=r2[xi][:], scalar=meta[:, t, 3:4], in1=x3[xi][:], op0=ALU.mult, op1=ALU.add), reads=[br12[xi], bmeta, bx3[xi]], writes=[bx3[xi]])
                fw.op("act", lambda xi=xi: A.activation(out=h2f[:], in_=x3[xi][:], func=AF.Square, accum_out=st3[:, 0:1]), reads=[bx3[xi]], writes=[bh2f, bst3])
                fw.op("act", lambda: A.activation(out=st3[:, 1:2], in_=st3[:, 0:1], func=AF.Ln, scale=1.0 / D, bias=EPS), reads=[bst3], writes=[bst3])
                fw.op("act", lambda: A.activation(out=st3[:, 2:3], in_=st3[:, 1:2], func=AF.Exp, scale=-0.5), reads=[bst3], writes=[bst3])
                fw.op("dve", lambda xi=xi: V.scalar_tensor_tensor(out=h2f[:], in0=x3[xi][:], scalar=st3[:, 2:3], in1=gfin_b[:], op0=ALU.mult, op1=ALU.mult), reads=[bx3[xi], bst3, b_p3], writes=[bh2f])
                if t < NT3 - 1:
                    out_evs.append(fw.dma("sp", lambda t=t: nc.sync.dma_start(out=y_own[t * 128:(t + 1) * 128, :], in_=h2f[:]), reads=[bh2f]))
                else:
                    out_evs.append(fw.dma("sp", lambda: nc.sync.dma_start(out=ys[:, :], in_=h2f[0:NST, :]), reads=[bh2f]))
            fw.barrier()
        for ev in out_evs:
            fw._wait("sp", ev)
        print("instructions:", fw.nins)
    return nc


def kernel(x_prompt, x_sample, cache_k, cache_v, state_lru_h, state_conv, page_table,
           g_mix, w_in, conv_w, conv_b, w_a, b_a, w_i, b_i, lam, b_sb, g_out_lru, g_out_sb, w_out,
           g_ffn, w_rg, b_rg, w_re, b_re, w_eg, w_eu, w_ed, g_final):
    f32 = np.float32
    x_prompt = np.asarray(x_prompt, f32); x_sample = np.asarray(x_sample, f32)
    npool = cache_k.shape[1]
    ck = np.ascontiguousarray(np.asarray(cache_k, f32)[0].reshape(npool * PAGE, SBW))
    cv = np.ascontiguousarray(np.asarray(cache_v, f32)[0].reshape(npool * PAGE, SBW))
    shared = {"g_mix": g_mix[0], "w_in": w_in[0], "conv_w": conv_w[0], "conv_b": conv_b[0], "w_a": w_a[0], "b_a": b_a[0],
              "w_i": w_i[0], "b_i": b_i[0], "lam": lam[0], "b_sb": b_sb[0], "g_out_lru": g_out_lru[0], "g_out_sb": g_out_sb[0],
              "w_out": w_out[0], "g_ffn": g_ffn[0], "w_rg": w_rg[0], "b_rg": b_rg[0], "w_re": w_re[0], "b_re": b_re[0],
              "w_eg": w_eg[0], "w_eu": w_eu[0], "w_ed": w_ed[0], "g_final": g_final, "cache_k": ck, "cache_v": cv}
    shared = {k: np.ascontiguousarray(np.asarray(v, f32)) for k, v in shared.items()}
    in_maps = []
    for c in range(8):
        b, j = c // 2, c % 2
        x = x_prompt[b]
        xq = x if j == 1 else np.concatenate([np.zeros((128, D), f32), x[:-128]], axis=0)
        m = dict(shared)
        m["xq"] = np.ascontiguousarray(xq)
        m["valid"] = np.full((128, 1), float(j), f32)
        sl = slice(NS * c, NS * c + NS)
        m["xs"] = np.ascontiguousarray(x_sample[sl].reshape(NST, D))
        m["st_h"] = np.ascontiguousarray(np.asarray(state_lru_h, f32)[0][sl])
        m["st_conv"] = np.ascontiguousarray(np.asarray(state_conv, f32)[0][sl])
        m["pt"] = np.ascontiguousarray(np.asarray(page_table)[sl].astype(np.int32))
        in_maps.append(m)
    nc = build({"npool": npool})
    res = run_bass_kernel_spmd(nc, in_maps, core_ids=list(range(8)))
    r = res.results
    B = x_prompt.shape[0]
    y_prompt = np.zeros((B, T, D), f32); y_sample = np.zeros((8 * NS, 4, D), f32)
    k_prompt = np.zeros((1, B, T, NH, HD), f32); v_prompt = np.zeros((1, B, T, NH, HD), f32)
    lru_h_prompt = np.zeros((1, B, LW), f32); conv_prompt = np.zeros((1, B, 3, LW), f32)
    k_sample = np.zeros((1, 8 * NS, 4, NH, HD), f32); v_sample = np.zeros((1, 8 * NS, 4, NH, HD), f32)
    lru_h_sample = np.zeros((1, 8 * NS, LW), f32); conv_sample = np.zeros((1, 8 * NS, 3, LW), f32)
    for c in range(8):
        b, j = c // 2, c % 2
        y_prompt[b].reshape(16, 2, 128, D)[:, j] = r[c]["y_own"].reshape(16, 128, D)
        sl = slice(NS * c, NS * c + NS)
        y_sample[sl] = r[c]["ys"].reshape(NS, 4, D)
        k_sample[0, sl] = r[c]["ks"].reshape(NS, 4, NH, HD); v_sample[0, sl] = r[c]["vs"].reshape(NS, 4, NH, HD)
        lru_h_sample[0, sl] = r[c]["lru_hs"]; conv_sample[0, sl] = r[c]["convs"]
        if j == 1:
            k_prompt[0, b] = r[c]["k_all"].reshape(T, NH, HD); v_prompt[0, b] = r[c]["v_all"].reshape(T, NH, HD)
            lru_h_prompt[0, b] = r[c]["lru_h"]; conv_prompt[0, b] = r[c]["convb"]
    return (y_prompt, y_sample, k_prompt, v_prompt, lru_h_prompt, conv_prompt, k_sample, v_sample, lru_h_sample, conv_sample)
```
